# Optimizing a Trainium2 kernel written in Bass

```python
import math
import jax, jax.numpy as jnp
from jax import lax
import numpy as np

D_MODEL = 1024
BATCH = 1
SEQ = 16384
DEPTH = 4

GRID_W = 64
CTX_LEN = 256
EPS = 1e-6
ROPE_THETA = 10000.0
Q_BLOCK = 128
N_BRANCH = 3

MLA_HEADS = 8
MLA_Q_RANK = 256
MLA_KV_RANK = 128
MLA_NOPE = 64
MLA_ROPE = 32
MLA_V = 64
MLA_QK = MLA_NOPE + MLA_ROPE
MLA_WIDTH = MLA_HEADS * MLA_V

DIF_HEADS = 4
DIF_HEAD_DIM = 64
DIF_WIDTH = DIF_HEADS * 2 * DIF_HEAD_DIM

SSM_HEADS = 8
SSM_HEAD_DIM = 64
SSM_WIDTH = SSM_HEADS * SSM_HEAD_DIM
SSM_GROUPS = 2
SSM_STATE = 128
SSM_CONV = 5
SSM_CHUNK = 128

N_EXPERTS = 32
TOP_K = 4
D_FF = 1024
SWIGLU_LIMIT = 7.0
SWIGLU_ALPHA = 1.702
MOE_BLOCK = 256

MLA_COLS = MLA_Q_RANK + MLA_KV_RANK + MLA_ROPE
DIF_COLS = 3 * DIF_WIDTH
SSM_COLS = SSM_WIDTH + SSM_WIDTH + 2 * SSM_GROUPS * SSM_STATE + 2 * SSM_HEADS
GATE_COLS = N_BRANCH * D_MODEL
IN_COLS = MLA_COLS + DIF_COLS + SSM_COLS + GATE_COLS

kernel_name = 'hybrid_mla_diff_ssd_moe_diffusion_trunk'


def _rms(x, g):
    xf = x.astype(jnp.float32)
    y = xf * lax.rsqrt(jnp.mean(xf * xf, axis=-1, keepdims=True) + EPS)
    return (y * g.astype(jnp.float32)).astype(x.dtype)


def _modulate(x, g, shift, scale):
    return _rms(x, g) * (1.0 + scale) + shift


def _split(x, sizes):
    idx = np.cumsum(sizes)[:-1].tolist()
    return jnp.split(x, idx, axis=-1)


def _axial_rope_tables(seq_len, rot_dim):
    n_rows = seq_len // GRID_W
    row = jnp.repeat(jnp.arange(n_rows), GRID_W).astype(jnp.float32)
    col = jnp.tile(jnp.arange(GRID_W), n_rows).astype(jnp.float32)
    axis_dim = rot_dim // 2
    inv = ROPE_THETA ** (-jnp.arange(0, axis_dim, 2, dtype=jnp.float32) / axis_dim)
    ang_r = row[:, None] * inv
    ang_c = col[:, None] * inv
    return (jnp.cos(ang_r), jnp.sin(ang_r), jnp.cos(ang_c), jnp.sin(ang_c))


def _rotate(x, cos, sin):
    shape = (x.shape[1],) + (1,) * (x.ndim - 3) + (cos.shape[-1],)
    cos = cos.reshape(shape)
    sin = sin.reshape(shape)
    x1, x2 = jnp.split(x.astype(jnp.float32), 2, axis=-1)
    return jnp.concatenate([x1 * cos - x2 * sin, x2 * cos + x1 * sin], axis=-1).astype(x.dtype)


def _axial_rope(x, tabs):
    cr, sr, cc, sc = tabs
    xr, xc = jnp.split(x, 2, axis=-1)
    return jnp.concatenate([_rotate(xr, cr, sr), _rotate(xc, cc, sc)], axis=-1)


def _sweep_query_blocks(step, q):
    bsz, s = q.shape[:2]
    nb = s // Q_BLOCK
    qb = jnp.swapaxes(q.reshape((bsz, nb, Q_BLOCK) + q.shape[2:]), 0, 1)
    out = lax.map(step, qb)
    return jnp.swapaxes(out, 0, 1).reshape((bsz, s) + out.shape[3:])


def _softmax_attend(q, k, v):
    s = jnp.einsum('bqhd,bkhd->bhqk', q, k).astype(jnp.float32) * (q.shape[-1] ** -0.5)
    p = jax.nn.softmax(s, axis=-1).astype(v.dtype)
    return jnp.einsum('bhqk,bkhe->bqhe', p, v)


def _diff_attend(q, k, v, lam):
    s = jnp.einsum('bqhmd,bkhmd->bmhqk', q, k).astype(jnp.float32) * (q.shape[-1] ** -0.5)
    p = jax.nn.softmax(s, axis=-1)
    w = (p[:, 0] - lam * p[:, 1]).astype(v.dtype)
    return jnp.einsum('bhqk,bkhe->bqhe', w, v)


def _rope_tail(t, tabs, n_rot):
    return jnp.concatenate([t[..., :-n_rot], _axial_rope(t[..., -n_rot:], tabs)], axis=-1)


def _mla_q(cq, g_q, w_uq, g_qn):
    q = _rms(cq, g_q) @ w_uq
    q = q.reshape(q.shape[:2] + (MLA_HEADS, MLA_QK))
    return _rms(q, g_qn)


def _mla_kv(ckv, k_rope, g_kv, w_ukv, g_kn):
    kv = (_rms(ckv, g_kv) @ w_ukv).reshape(ckv.shape[:2] + (MLA_HEADS, MLA_NOPE + MLA_V))
    k_nope, v = jnp.split(kv, [MLA_NOPE], axis=-1)
    k_pe = jnp.broadcast_to(k_rope[:, :, None, :], k_nope.shape[:3] + (MLA_ROPE,))
    k = _rms(jnp.concatenate([k_nope, k_pe], axis=-1), g_kn)
    return k, v


def _mla_branch(cols_ctx, cols_lat, rope, g_q, w_uq, g_kv, w_ukv, g_qn, g_kn, emit_ctx):
    cq_l, ckv_l, kr_l = _split(cols_lat, [MLA_Q_RANK, MLA_KV_RANK, MLA_ROPE])
    cq_c, ckv_c, kr_c = _split(cols_ctx, [MLA_Q_RANK, MLA_KV_RANK, MLA_ROPE])
    k_c, v_c = _mla_kv(ckv_c, kr_c, g_kv, w_ukv, g_kn)
    q_l = _rope_tail(_mla_q(cq_l, g_q, w_uq, g_qn), rope, MLA_ROPE)
    k_l, v_l = _mla_kv(ckv_l, kr_l, g_kv, w_ukv, g_kn)
    k_l = _rope_tail(k_l, rope, MLA_ROPE)
    k_all = jnp.concatenate([k_l, k_c], axis=1)
    v_all = jnp.concatenate([v_l, v_c], axis=1)
    y_l = _sweep_query_blocks(lambda qb: _softmax_attend(qb, k_all, v_all), q_l)
    y_l = y_l.reshape(y_l.shape[:2] + (MLA_WIDTH,))
    y_c = None
    if emit_ctx:
        y_c = _softmax_attend(_mla_q(cq_c, g_q, w_uq, g_qn), k_c, v_c)
        y_c = y_c.reshape(y_c.shape[:2] + (MLA_WIDTH,))
    return y_l, y_c


def _diff_qk(t, g, rope):
    t = _rms(t.reshape(t.shape[:2] + (DIF_HEADS, 2, DIF_HEAD_DIM)), g)
    return t if rope is None else _axial_rope(t, rope)


def _diff_v(t):
    return t.reshape(t.shape[:2] + (DIF_HEADS, 2 * DIF_HEAD_DIM))


def _diff_branch(cols_ctx, cols_lat, rope, g_qn, g_kn, lam_p, g_sub, lam_init, emit_ctx):
    q_l, k_l, v_l = _split(cols_lat, [DIF_WIDTH] * 3)
    q_c, k_c, v_c = _split(cols_ctx, [DIF_WIDTH] * 3)
    lp = lam_p.astype(jnp.float32)
    lam = jnp.exp(jnp.sum(lp[0] * lp[1])) - jnp.exp(jnp.sum(lp[2] * lp[3])) + lam_init
    k_c = _diff_qk(k_c, g_kn, None)
    v_c = _diff_v(v_c)
    q_l = _diff_qk(q_l, g_qn, rope)
    k_l = _diff_qk(k_l, g_kn, rope)
    k_all = jnp.concatenate([k_l, k_c], axis=1)
    v_all = jnp.concatenate([_diff_v(v_l), v_c], axis=1)

    def finish(o):
        return (_rms(o, g_sub) * (1.0 - lam_init)).reshape(o.shape[:2] + (DIF_WIDTH,))

    y_l = finish(_sweep_query_blocks(lambda qb: _diff_attend(qb, k_all, v_all, lam), q_l))
    y_c = finish(_diff_attend(_diff_qk(q_c, g_qn, None), k_c, v_c, lam)) if emit_ctx else None
    return y_l, y_c


def _dwconv_centred(u, w, b):
    ch = u.shape[-1]
    k = w.shape[0]
    y = lax.conv_general_dilated(u, w[:, None, :], window_strides=(1,),
                                 padding=[(k // 2, k // 2)],
                                 dimension_numbers=('NWC', 'WIO', 'NWC'),
                                 feature_group_count=ch)
    return y + b


def _ssd(xs, dt, a, bm, cm, h0):
    bsz, seq, nh, hd = xs.shape
    nc = seq // SSM_CHUNK
    xd = (xs * dt[..., None]).reshape(bsz, nc, SSM_CHUNK, nh, hd)
    bm = bm.reshape(bsz, nc, SSM_CHUNK, nh, SSM_STATE)
    cm = cm.reshape(bsz, nc, SSM_CHUNK, nh, SSM_STATE)
    a_cum = jnp.cumsum(jnp.moveaxis((dt * a).reshape(bsz, nc, SSM_CHUNK, nh), 3, 1), axis=-1)
    causal = jnp.tril(jnp.ones((SSM_CHUNK, SSM_CHUNK), bool))
    seg = jnp.exp(jnp.where(causal, a_cum[..., :, None] - a_cum[..., None, :], -jnp.inf))
    scores = jnp.einsum('bclhn,bcshn->bhcls', cm, bm) * seg
    y_diag = jnp.einsum('bhcls,bcshp->bclhp', scores, xd)
    decay_to_end = jnp.exp(a_cum[..., -1:] - a_cum)
    chunk_states = jnp.einsum('bclhn,bhcl,bclhp->bchpn', bm, decay_to_end, xd)
    chunk_decay = jnp.exp(a_cum[..., -1])

    def carry_state(h, inp):
        s_c, d_c = inp
        return (h * d_c[..., None, None] + s_c).astype(h.dtype), h

    h_final, h_in = lax.scan(carry_state, h0,
                             (jnp.moveaxis(chunk_states, 1, 0), jnp.moveaxis(chunk_decay, 2, 0)))
    y_off = jnp.einsum('bclhn,bchpn,bhcl->bclhp', cm, jnp.moveaxis(h_in, 0, 1), jnp.exp(a_cum))
    return (y_diag + y_off).reshape(bsz, seq, nh, hd), h_final


def _ssm_inputs(cols, conv_w, conv_b, dt_bias):
    z, xbc, dt = _split(cols, [SSM_WIDTH, SSM_WIDTH + 2 * SSM_GROUPS * SSM_STATE, 2 * SSM_HEADS])
    xbc = jax.nn.silu(_dwconv_centred(xbc, conv_w, conv_b))
    xs, bs, cs = _split(xbc, [SSM_WIDTH, SSM_GROUPS * SSM_STATE, SSM_GROUPS * SSM_STATE])
    bsz, l = cols.shape[:2]
    rep = SSM_HEADS // SSM_GROUPS
    xs = xs.reshape(bsz, l, SSM_HEADS, SSM_HEAD_DIM)
    bs = jnp.repeat(bs.reshape(bsz, l, SSM_GROUPS, SSM_STATE), rep, axis=2)
    cs = jnp.repeat(cs.reshape(bsz, l, SSM_GROUPS, SSM_STATE), rep, axis=2)
    dt = jax.nn.softplus(dt.reshape(bsz, l, 2, SSM_HEADS) + dt_bias)
    return z, xs, bs, cs, dt


def _flip_if(t, rev):
    return jnp.flip(t, axis=1) if rev else t


def _gated_group_norm(y, z, g):
    bsz, l = y.shape[:2]
    y = y.reshape(bsz, l, SSM_WIDTH) * jax.nn.silu(z)
    y = _rms(y.reshape(bsz, l, SSM_GROUPS, SSM_WIDTH // SSM_GROUPS), g.reshape(SSM_GROUPS, -1))
    return y.reshape(bsz, l, SSM_WIDTH)


def _ssm_branch(cols_ctx, cols_lat, conv_w, conv_b, dt_bias, a_log, d_skip, g_norm, emit_ctx):
    zc, xc, bc, cc, dtc = _ssm_inputs(cols_ctx, conv_w, conv_b, dt_bias)
    zl, xl, bl, cl, dtl = _ssm_inputs(cols_lat, conv_w, conv_b, dt_bias)
    a = -jnp.exp(a_log)
    h0 = jnp.zeros((xl.shape[0], SSM_HEADS, SSM_HEAD_DIM, SSM_STATE), xl.dtype)
    y_l = 0.0
    y_c = 0.0
    for d in range(2):
        rev = d == 1
        skip = d_skip[d][None, None, :, None]
        yc_d, hc = _ssd(_flip_if(xc, rev), _flip_if(dtc[:, :, d], rev), a[d],
                        _flip_if(bc, rev), _flip_if(cc, rev), h0)
        yl_d, _ = _ssd(_flip_if(xl, rev), _flip_if(dtl[:, :, d], rev), a[d],
                       _flip_if(bl, rev), _flip_if(cl, rev), hc)
        y_l = y_l + _flip_if(yl_d, rev) + skip * xl
        y_c = y_c + _flip_if(yc_d, rev) + skip * xc
    out_l = _gated_group_norm(y_l, zl, g_norm)
    out_c = _gated_group_norm(y_c, zc, g_norm) if emit_ctx else None
    return out_l, out_c


def _merge(ya, yb, yc, gate_cols, p):
    g = jax.nn.sigmoid(gate_cols + p['b_gate'])
    g = g.reshape(gate_cols.shape[:-1] + (N_BRANCH, D_MODEL))
    m = (g[..., 0, :] * (ya @ p['w_up_mla']) + g[..., 1, :] * (yb @ p['w_up_dif'])
         + g[..., 2, :] * (yc @ p['w_up_ssm']))
    return m @ p['w_out']


def _moe(h, w_router, b_router, w_gu, b_gu, w_down, b_down):
    n_tok, dm = h.shape
    logits = (h @ w_router + b_router).astype(jnp.float32)
    top_val, top_idx = lax.top_k(logits, TOP_K)
    gates = jax.nn.softmax(top_val, axis=-1).astype(h.dtype)
    n_assign = n_tok * TOP_K
    flat_e = top_idx.reshape(-1).astype(jnp.int32)
    flat_t = jnp.arange(n_assign, dtype=jnp.int32) // TOP_K
    flat_g = gates.reshape(-1)
    counts = jnp.zeros((N_EXPERTS,), jnp.int32).at[flat_e].add(1)
    padded = (counts + MOE_BLOCK - 1) // MOE_BLOCK * MOE_BLOCK
    pad_end = jnp.cumsum(padded)
    pad_start = pad_end - padded
    raw_start = jnp.cumsum(counts) - counts
    order = jnp.argsort(flat_e)
    sorted_e = flat_e[order]
    dest = pad_start[sorted_e] + jnp.arange(n_assign, dtype=jnp.int32) - raw_start[sorted_e]
    n_blocks = -(-(n_assign + N_EXPERTS * (MOE_BLOCK - 1)) // MOE_BLOCK)
    n_slots = n_blocks * MOE_BLOCK
    slot_tok = jnp.full((n_slots,), n_tok, jnp.int32).at[dest].set(flat_t[order])
    slot_gate = jnp.zeros((n_slots,), h.dtype).at[dest].set(flat_g[order])
    block_start = jnp.arange(n_blocks, dtype=jnp.int32) * MOE_BLOCK
    block_e = jnp.minimum(jnp.searchsorted(pad_end, block_start, side='right'), N_EXPERTS - 1)
    h_pad = jnp.concatenate([h, jnp.zeros((1, dm), h.dtype)], axis=0)
    xb = h_pad[slot_tok].reshape(n_blocks, MOE_BLOCK, dm)

    def expert_block(args):
        xblk, e = args
        gu = xblk @ w_gu[e] + b_gu[e]
        glu, lin = jnp.split(gu, 2, axis=-1)
        glu = jnp.minimum(glu, SWIGLU_LIMIT)
        lin = jnp.clip(lin, -SWIGLU_LIMIT, SWIGLU_LIMIT)
        act = glu * jax.nn.sigmoid(SWIGLU_ALPHA * glu) * (lin + 1.0)
        return act @ w_down[e] + b_down[e]

    yb = lax.map(expert_block, (xb, block_e))
    y_slots = yb.reshape(n_slots, dm) * slot_gate[:, None]
    return jnp.zeros((n_tok + 1, dm), h.dtype).at[slot_tok].add(y_slots)[:n_tok]


def _layer(x_lat, x_ctx, c, c_ctx, rope_mla, rope_dif, lam_init, emit_ctx, p):
    mod_l = (jax.nn.silu(c) @ p['w_mod'] + p['b_mod'])[:, None, :]
    mod_c = jax.nn.silu(c_ctx) @ p['w_mod'] + p['b_mod']
    sh1_l, sc1_l, gt1_l, sh2_l, sc2_l, gt2_l = jnp.split(mod_l, 6, axis=-1)
    sh1_c, sc1_c, gt1_c, sh2_c, sc2_c, gt2_c = jnp.split(mod_c, 6, axis=-1)

    h_l = _modulate(x_lat, p['g_norm1'], sh1_l, sc1_l)
    h_c = _modulate(x_ctx, p['g_norm1'], sh1_c, sc1_c)
    u_l = h_l @ p['w_in']
    u_c = h_c @ p['w_in']
    mla_l, dif_l, ssm_l, gate_l = _split(u_l, [MLA_COLS, DIF_COLS, SSM_COLS, GATE_COLS])
    mla_c, dif_c, ssm_c, gate_c = _split(u_c, [MLA_COLS, DIF_COLS, SSM_COLS, GATE_COLS])

    ya_l, ya_c = _mla_branch(mla_c, mla_l, rope_mla, p['mla_g_q'], p['mla_w_uq'], p['mla_g_kv'],
                             p['mla_w_ukv'], p['mla_g_qn'], p['mla_g_kn'], emit_ctx)
    yb_l, yb_c = _diff_branch(dif_c, dif_l, rope_dif, p['dif_g_qn'], p['dif_g_kn'],
                              p['dif_lambda'], p['dif_g_sub'], lam_init, emit_ctx)
    yc_l, yc_c = _ssm_branch(ssm_c, ssm_l, p['ssm_conv_w'], p['ssm_conv_b'], p['ssm_dt_bias'],
                             p['ssm_a_log'], p['ssm_d'], p['ssm_g_norm'], emit_ctx)

    x_lat = x_lat + gt1_l * _merge(ya_l, yb_l, yc_l, gate_l, p)
    f_l = _modulate(x_lat, p['g_norm2'], sh2_l, sc2_l)
    moe_args = (p['moe_w_router'], p['moe_b_router'], p['moe_w_gu'], p['moe_b_gu'],
                p['moe_w_down'], p['moe_b_down'])
    if emit_ctx:
        x_ctx = x_ctx + gt1_c * _merge(ya_c, yb_c, yc_c, gate_c, p)
        f_c = _modulate(x_ctx, p['g_norm2'], sh2_c, sc2_c)
        n_c = f_c.shape[0] * f_c.shape[1]
        y = _moe(jnp.concatenate([f_c.reshape(-1, D_MODEL), f_l.reshape(-1, D_MODEL)], axis=0), *moe_args)
        x_ctx = x_ctx + gt2_c * y[:n_c].reshape(x_ctx.shape)
        y_l = y[n_c:]
    else:
        y_l = _moe(f_l.reshape(-1, D_MODEL), *moe_args)
    x_lat = x_lat + gt2_l * y_l.reshape(x_lat.shape)
    return x_lat, x_ctx


def setup_inputs(seed: int = 0) -> dict:
    key = jax.random.key(seed)
    ks = iter(jax.random.split(key, 48))
    L = DEPTH

    def nrm(shape, scale):
        return jax.random.normal(next(ks), shape, jnp.float32) * scale

    def gain(shape):
        return 1.0 + nrm(shape, 0.05)

    x = nrm((BATCH, SEQ, D_MODEL), 1.0)
    c = nrm((BATCH, D_MODEL), 1.0)
    ctx = nrm((BATCH, CTX_LEN, D_MODEL), 1.0)
    c_ctx = nrm((D_MODEL,), 1.0)
    w_mod = nrm((L, D_MODEL, 6 * D_MODEL), 0.5 * D_MODEL ** -0.5)
    b_mod = nrm((L, 6 * D_MODEL), 0.02)
    g_norm1 = gain((L, D_MODEL))
    g_norm2 = gain((L, D_MODEL))
    w_in = nrm((L, D_MODEL, IN_COLS), D_MODEL ** -0.5)
    b_gate = nrm((L, GATE_COLS), 0.02)
    mla_g_q = gain((L, MLA_Q_RANK))
    mla_w_uq = nrm((L, MLA_Q_RANK, MLA_HEADS * MLA_QK), MLA_Q_RANK ** -0.5)
    mla_g_kv = gain((L, MLA_KV_RANK))
    mla_w_ukv = nrm((L, MLA_KV_RANK, MLA_HEADS * (MLA_NOPE + MLA_V)), MLA_KV_RANK ** -0.5)
    mla_g_qn = gain((L, MLA_QK))
    mla_g_kn = gain((L, MLA_QK))
    dif_g_qn = gain((L, DIF_HEAD_DIM))
    dif_g_kn = gain((L, DIF_HEAD_DIM))
    dif_lambda = nrm((L, 4, DIF_HEAD_DIM), 0.1)
    dif_g_sub = gain((L, 2 * DIF_HEAD_DIM))
    ssm_conv_w = nrm((L, SSM_CONV, SSM_WIDTH + 2 * SSM_GROUPS * SSM_STATE), SSM_CONV ** -0.5)
    ssm_conv_b = nrm((L, SSM_WIDTH + 2 * SSM_GROUPS * SSM_STATE), 0.02)
    dt0 = jnp.exp(jax.random.uniform(next(ks), (L, 2, SSM_HEADS), jnp.float32,
                                     minval=math.log(1e-3), maxval=math.log(1e-1)))
    ssm_dt_bias = dt0 + jnp.log(-jnp.expm1(-dt0))
    ssm_a_log = jnp.log(jax.random.uniform(next(ks), (L, 2, SSM_HEADS), jnp.float32,
                                           minval=1.0, maxval=16.0))
    ssm_d = gain((L, 2, SSM_HEADS))
    ssm_g_norm = gain((L, SSM_WIDTH))
    w_up_mla = nrm((L, MLA_WIDTH, D_MODEL), MLA_WIDTH ** -0.5)
    w_up_dif = nrm((L, DIF_WIDTH, D_MODEL), DIF_WIDTH ** -0.5)
    w_up_ssm = nrm((L, SSM_WIDTH, D_MODEL), SSM_WIDTH ** -0.5)
    w_out = nrm((L, D_MODEL, D_MODEL), D_MODEL ** -0.5)
    moe_w_router = nrm((L, D_MODEL, N_EXPERTS), D_MODEL ** -0.5)
    moe_b_router = nrm((L, N_EXPERTS), 0.01)
    moe_w_gu = nrm((L, N_EXPERTS, D_MODEL, 2 * D_FF), D_MODEL ** -0.5)
    moe_b_gu = nrm((L, N_EXPERTS, 2 * D_FF), 0.02)
    moe_w_down = nrm((L, N_EXPERTS, D_FF, D_MODEL), D_FF ** -0.5)
    moe_b_down = nrm((L, N_EXPERTS, D_MODEL), 0.02)
    return {'x': x, 'c': c, 'ctx': ctx, 'c_ctx': c_ctx, 'w_mod': w_mod, 'b_mod': b_mod,
            'g_norm1': g_norm1, 'g_norm2': g_norm2, 'w_in': w_in, 'b_gate': b_gate,
            'mla_g_q': mla_g_q, 'mla_w_uq': mla_w_uq, 'mla_g_kv': mla_g_kv, 'mla_w_ukv': mla_w_ukv,
            'mla_g_qn': mla_g_qn, 'mla_g_kn': mla_g_kn, 'dif_g_qn': dif_g_qn, 'dif_g_kn': dif_g_kn,
            'dif_lambda': dif_lambda, 'dif_g_sub': dif_g_sub, 'ssm_conv_w': ssm_conv_w,
            'ssm_conv_b': ssm_conv_b, 'ssm_dt_bias': ssm_dt_bias, 'ssm_a_log': ssm_a_log,
            'ssm_d': ssm_d, 'ssm_g_norm': ssm_g_norm, 'w_up_mla': w_up_mla, 'w_up_dif': w_up_dif,
            'w_up_ssm': w_up_ssm, 'w_out': w_out, 'moe_w_router': moe_w_router,
            'moe_b_router': moe_b_router, 'moe_w_gu': moe_w_gu, 'moe_b_gu': moe_b_gu,
            'moe_w_down': moe_w_down, 'moe_b_down': moe_b_down}


def reference(x, c, ctx, c_ctx, w_mod, b_mod, g_norm1, g_norm2, w_in, b_gate, mla_g_q, mla_w_uq,
              mla_g_kv, mla_w_ukv, mla_g_qn, mla_g_kn, dif_g_qn, dif_g_kn, dif_lambda, dif_g_sub,
              ssm_conv_w, ssm_conv_b, ssm_dt_bias, ssm_a_log, ssm_d, ssm_g_norm, w_up_mla, w_up_dif,
              w_up_ssm, w_out, moe_w_router, moe_b_router, moe_w_gu, moe_b_gu, moe_w_down,
              moe_b_down):
    rope_mla = _axial_rope_tables(x.shape[1], MLA_ROPE)
    rope_dif = _axial_rope_tables(x.shape[1], DIF_HEAD_DIM)
    x_lat, x_ctx = x, ctx
    for i in range(DEPTH):
        p = {'w_mod': w_mod[i], 'b_mod': b_mod[i], 'g_norm1': g_norm1[i], 'g_norm2': g_norm2[i],
             'w_in': w_in[i], 'b_gate': b_gate[i], 'mla_g_q': mla_g_q[i], 'mla_w_uq': mla_w_uq[i],
             'mla_g_kv': mla_g_kv[i], 'mla_w_ukv': mla_w_ukv[i], 'mla_g_qn': mla_g_qn[i],
             'mla_g_kn': mla_g_kn[i], 'dif_g_qn': dif_g_qn[i], 'dif_g_kn': dif_g_kn[i],
             'dif_lambda': dif_lambda[i], 'dif_g_sub': dif_g_sub[i], 'ssm_conv_w': ssm_conv_w[i],
             'ssm_conv_b': ssm_conv_b[i], 'ssm_dt_bias': ssm_dt_bias[i], 'ssm_a_log': ssm_a_log[i],
             'ssm_d': ssm_d[i], 'ssm_g_norm': ssm_g_norm[i], 'w_up_mla': w_up_mla[i],
             'w_up_dif': w_up_dif[i], 'w_up_ssm': w_up_ssm[i], 'w_out': w_out[i],
             'moe_w_router': moe_w_router[i], 'moe_b_router': moe_b_router[i],
             'moe_w_gu': moe_w_gu[i], 'moe_b_gu': moe_b_gu[i], 'moe_w_down': moe_w_down[i],
             'moe_b_down': moe_b_down[i]}
        lam_init = 0.8 - 0.6 * math.exp(-0.3 * i)
        x_lat, x_ctx = _layer(x_lat, x_ctx, c, c_ctx, rope_mla, rope_dif, lam_init,
                              i < DEPTH - 1, p)
    return x_lat
```

```python
import contextlib
import math
import numpy as np
import ml_dtypes
import concourse.bass as bass
import concourse.mybir as mybir
from concourse.bass_utils import run_bass_kernel_spmd

F32 = mybir.dt.float32
BF16 = mybir.dt.bfloat16
AF = mybir.ActivationFunctionType
ALU = mybir.AluOpType
AX = mybir.AxisListType

D = 1024
NCORE = 8
TC = 256
DEPTH = 4
EPS = 1e-6
GRID_W = 64
N_DMA_SEM = 6
NEXP = 32
DFF = 1024

MLA_COLS = 416
DIF0 = 416
SSM0 = 1952
Z0, XBC0, DT0, GATE0 = 1952, 2464, 3488, 3504
IN_COLS = 6576


class Prog:
    ENGS = ("pe", "act", "dve", "pool", "sp")

    def __init__(self, nc):
        self.nc = nc
        self.ops = []
        self.stack = contextlib.ExitStack()
        self.levels = [self.stack]
        self.barriers = []
        self.prefix = ""

    def push(self):
        self.levels.append(contextlib.ExitStack())

    def pop(self):
        self.barrier()
        self.levels.pop().close()

    def sb(self, name, shape, dt=F32):
        return self.levels[-1].enter_context(self.nc.sbuf_tensor("sb_" + self.prefix + name, list(shape), dt))

    def ps(self, name, shape, dt=F32):
        return self.stack.enter_context(self.nc.psum_tensor(name, list(shape), dt))

    def din(self, name, shape, dt=F32):
        return self.nc.dram_tensor(name, list(shape), dt, kind="ExternalInput").ap()

    def dout(self, name, shape, dt=F32):
        return self.nc.dram_tensor(name, list(shape), dt, kind="ExternalOutput").ap()

    def dram(self, name, shape, dt=F32):
        return self.nc.dram_tensor(name, list(shape), dt, kind="Internal").ap()

    @staticmethod
    def _names(aps):
        out = []
        for a in aps:
            if a is None or isinstance(a, (int, float)):
                continue
            out.append(a.name)
        return out

    def add(self, eng, fn, reads, writes, is_dma=False):
        self.ops.append((eng, fn, self._names(reads), self._names(writes), is_dma))

    def barrier(self):
        self.barriers.append(len(self.ops))

    def mm(self, out, lhsT, rhs, start=True, stop=True):
        self.add("pe", lambda e: e.matmul(out, lhsT, rhs, start=start, stop=stop), [lhsT, rhs], [out])

    def tr(self, out, in_, ident):
        self.add("pe", lambda e: e.transpose(out, in_, ident), [in_, ident], [out])

    def act(self, out, in_, func, bias=None, scale=1.0):
        kw = {}
        if bias is not None:
            kw["bias"] = bias
        self.add("act", lambda e: e.activation(out, in_, func, scale=scale, **kw), [in_, bias, scale], [out])

    def tt(self, out, in0, in1, op, eng="dve"):
        self.add(eng, lambda e: e.tensor_tensor(out, in0, in1, op), [in0, in1], [out])

    def ts(self, out, in0, s1, s2, op0, op1=None, eng="dve"):
        kw = {}
        if op1 is not None:
            kw["op1"] = op1
        self.add(eng, lambda e: e.tensor_scalar(out, in0, s1, s2, op0, **kw), [in0, s1, s2], [out])

    def stt(self, out, in0, scalar, in1, op0, op1, eng="dve"):
        self.add(eng, lambda e: e.scalar_tensor_tensor(out, in0, scalar, in1, op0, op1), [in0, scalar, in1], [out])

    def copy(self, out, in_, eng="dve"):
        if eng == "act":
            self.add("act", lambda e: e.copy(out, in_), [in_], [out])
        else:
            self.add(eng, lambda e: e.tensor_copy(out, in_), [in_], [out])

    def memset(self, ap, val, eng="dve"):
        self.add(eng, lambda e: e.memset(ap, val), [], [ap])

    def reduce(self, out, in_, op=None, eng="dve"):
        op = op or ALU.add
        self.add(eng, lambda e: e.tensor_reduce(out, in_, AX.X, op), [in_], [out])

    def recip(self, out, in_):
        self.add("dve", lambda e: e.reciprocal(out, in_), [in_], [out])

    def dma(self, out, in_, q="sp", **kw):
        self.add(q, lambda e: e.dma_start(out=out, in_=in_, **kw), [in_], [out], is_dma=True)

    def cc(self, kind, op, in_ap, out_ap):
        rg = [list(range(NCORE))]
        self.add("pool", lambda e: e.collective_compute(kind, op, replica_groups=rg, ins=[in_ap.opt()],
                                                        outs=[out_ap.opt()]), [in_ap], [out_ap], is_dma="cc")

    def rstd(self, out, ssq, n):
        self.act(out, ssq, AF.Sqrt, bias=EPS, scale=1.0 / n)
        self.recip(out, out)

    def emit(self):
        nc = self.nc
        ops = self.ops
        n = len(ops)
        eng_cnt = {e: 0 for e in self.ENGS}
        dma_cnt = {e: 0 for e in self.ENGS}
        done = [None] * n
        dma_prev = [None] * n
        cc_cnt = 0
        last_cc = None
        for i, (eng, fn, r, w, is_dma) in enumerate(ops):
            if is_dma == "cc":
                cc_cnt += 1
                done[i] = (("cc",), cc_cnt)
            elif is_dma:
                k = dma_cnt[eng]
                dma_cnt[eng] += 1
                key = ("dma", eng, k % N_DMA_SEM)
                done[i] = (key, 16 * (k // N_DMA_SEM + 1))
                if k >= N_DMA_SEM:
                    dma_prev[i] = (key, 16 * (k // N_DMA_SEM))
            else:
                eng_cnt[eng] += 1
                done[i] = (("eng", eng), eng_cnt[eng])
        last_w = {}
        readers = {}
        deps = [None] * n
        bar_set = set(self.barriers)
        last_eng_op = {}
        dmas_since = []
        pending_bar = {}
        for i, (eng, fn, r, w, is_dma) in enumerate(ops):
            if i in bar_set:
                bd = set(last_eng_op.values()) | set(dmas_since)
                dmas_since = []
                for e in self.ENGS:
                    pending_bar[e] = pending_bar.get(e, set()) | bd
            d = set()
            if eng in pending_bar:
                d |= pending_bar.pop(eng)
            for t in r:
                if t in last_w:
                    d.add(last_w[t])
            for t in w:
                if t in last_w:
                    d.add(last_w[t])
                for j in readers.get(t, ()):
                    d.add(j)
            if is_dma == "cc":
                if last_cc is not None:
                    d.add(last_cc)
                last_cc = i
            d.discard(i)
            deps[i] = d
            for t in w:
                last_w[t] = i
                readers[t] = []
            for t in r:
                if t not in w:
                    lst = readers.setdefault(t, [])
                    lst.append(i)
                    if len(lst) > 48:
                        keep = {}
                        rest = []
                        for j in lst:
                            if ops[j][4]:
                                rest.append(j)
                            else:
                                keep[ops[j][0]] = j
                        readers[t] = rest + list(keep.values())
            if is_dma:
                dmas_since.append(i)
            else:
                last_eng_op[eng] = i
        sem_keys = [("eng", e) for e in self.ENGS if eng_cnt[e] > 0]
        for e in self.ENGS:
            for k in range(min(dma_cnt[e], N_DMA_SEM)):
                sem_keys.append(("dma", e, k))
        if cc_cnt:
            sem_keys.append(("cc",))
        sems = {}
        for key in sem_keys:
            sems[key] = self.stack.enter_context(nc.semaphore("s_" + "_".join(str(x) for x in key)))
        per_eng = {e: [] for e in self.ENGS}
        for i, op in enumerate(ops):
            per_eng[op[0]].append(i)
        final_vals = {}
        for i in range(n):
            key, val = done[i]
            final_vals[key] = max(final_vals.get(key, 0), val)

        def emit_engine(eng, e):
            waited = {}
            for i in per_eng[eng]:
                _, fn, r, w, is_dma = ops[i]
                need = {}
                for j in deps[i]:
                    if ops[j][0] == "pe" and eng == "pe" and not is_dma and not ops[j][4]:
                        continue
                    key, val = done[j]
                    need[key] = max(need.get(key, 0), val)
                if dma_prev[i] is not None:
                    key, val = dma_prev[i]
                    need[key] = max(need.get(key, 0), val)
                for key, val in need.items():
                    if waited.get(key, 0) >= val:
                        continue
                    e.wait_ge(sems[key], val)
                    waited[key] = val
                ins = fn(e)
                key, val = done[i]
                ins.then_inc(sems[key], 16 if is_dma is True else 1)
            if eng == "sp":
                for key, val in final_vals.items():
                    e.wait_ge(sems[key], val)

        with nc.Block() as block:
            @block.tensor
            def _(e):
                emit_engine("pe", e)

            @block.scalar
            def _(e):
                emit_engine("act", e)

            @block.vector
            def _(e):
                emit_engine("dve", e)

            @block.gpsimd
            def _(e):
                emit_engine("pool", e)

            @block.sync
            def _(e):
                emit_engine("sp", e)
        self.stack.close()


def bc(ap, shape):
    return ap.to_broadcast(list(shape))


def rope_ops(p, dst, src, cos_t, sin_t, G, half, t1, t2):
    W = 4 * half
    p.tt(t1, src, bc(cos_t.unsqueeze(1), [128, G, W]), ALU.mult)
    for a in range(2):
        for b in range(2):
            o = a * 2 * half + b * half
            s = a * 2 * half + (1 - b) * half
            p.tt(t2[:, :, o:o + half], src[:, :, s:s + half],
                 bc(sin_t[:, o:o + half].unsqueeze(1), [128, G, half]), ALU.mult)
    p.tt(dst, t1, t2, ALU.add)


def consts_np():
    t = np.arange(128)
    U = (t[:, None] <= t[None, :]).astype(np.float32)
    return np.stack([np.eye(128, dtype=np.float32), U, U.T.copy(), np.ones((128, 128), np.float32)])


def rope_tables_np(seq):
    n_rows = seq // GRID_W
    row = np.repeat(np.arange(n_rows), GRID_W).astype(np.float32)
    col = np.tile(np.arange(GRID_W), n_rows).astype(np.float32)
    outs = []
    for rot in (32, 64):
        ad = rot // 2
        inv = (10000.0 ** (-np.arange(0, ad, 2, dtype=np.float32) / ad)).astype(np.float32)
        ar = row[:, None] * inv
        ac = col[:, None] * inv
        cr, sr, cc_, sc_ = np.cos(ar), np.sin(ar), np.cos(ac), np.sin(ac)
        outs.append(np.concatenate([cr, cr, cc_, cc_], 1))
        outs.append(np.concatenate([-sr, sr, -sc_, sc_], 1))
    return np.concatenate(outs, 1).astype(np.float32)


def pack_small(inp, l):
    v = np.zeros((1, 1024), np.float32)
    o = 0
    for key, n in (("mla_g_q", 256), ("mla_g_kv", 128), ("mla_g_qn", 96), ("mla_g_kn", 96),
                   ("dif_g_qn", 64), ("dif_g_kn", 64), ("ssm_dt_bias", 16), ("ssm_a_log", 16), ("ssm_d", 16)):
        v[0, o:o + n] = np.asarray(inp[key][l]).reshape(-1)
        o += n
    return v


def token_groups(TL):
    gr = [(0, TC, True)]
    for g0 in range(0, TL, 512):
        gr.append((TC + g0, min(512, TL - g0), False))
    return gr


def build_fused(TL, depth):
    nc = bass.Bass("TRN2", target_bir_lowering=False)
    p = Prog(nc)
    T = TC + TL
    NT = T // 128
    TH = T + 128
    S = TL * NCORE
    NK = S + TC
    NKB = NK // 128
    NBL = TL // 128
    groups = token_groups(TL)
    x_in = p.din("x", [TL, D])
    ctx_in = p.din("ctx", [TC, D])
    cc_in = p.din("cc", [128, 8, 2])
    wmod_in = p.din("wmod_sh", [D, 3072])
    bmod_in = p.din("bmod_sh", [1, 3072])
    g1_in = p.din("g_norm1", [depth, D])
    g2_in = p.din("g_norm2", [depth, D])
    win_in = p.din("w_in", [depth, D, IN_COLS])
    wuq_in = p.din("w_uq", [depth, 256, 768])
    wukv_in = p.din("w_ukv", [depth, 128, 1024])
    small_in = p.din("small", [depth, 1024])
    convw_in = p.din("conv_w", [depth, 128, 8, 5])
    convb_in = p.din("conv_b", [depth, 128, 8])
    consts_in = p.din("consts", [4, 128, 128])
    rope_in = p.din("rope", [TL, 192])
    hmask_in = p.din("hmask", [1, 4])
    halosel_in = p.din("halosel", [32, 128])
    cmask_in = p.din("cmask", [1, 16])
    lam_in = p.din("lamc", [depth, 2])
    dlam_in = p.din("dif_lambda", [depth, 256])
    gsub_in = p.din("g_sub", [depth, 128, 1])
    gssm_in = p.din("g_ssm", [depth, 512])
    bgate_in = p.din("b_gate", [depth, 3072])
    wupa_in = p.din("w_up_mla", [depth, 512, D])
    wupd_in = p.din("w_up_dif", [depth, 512, D])
    wups_in = p.din("w_up_ssm", [depth, 512, D])
    wout_in = p.din("w_out", [depth, D, D])
    wr_in = p.din("w_router", [depth, D, NEXP])
    br_in = p.din("b_router", [depth, NEXP])
    wgu_in = p.din("w_gu", [depth, 4, D, 2 * DFF])
    bgu_in = p.din("b_gu", [depth, 128, 4, 16])
    wdn_in = p.din("w_down", [depth, 4, DFF, D])
    bdn_in = p.din("b_down_pad", [depth, NEXP, D])
    sele_in = p.din("sele", [NEXP, 4, 128])
    out = p.dout("out", [TL, D])
    XCUR = p.dram("XCUR", [TH, D])
    HB_in = p.dram("HB_in", [4, D])
    HB_all = p.dram("HB_all", [32, D])
    MODS_in = p.dram("MODS_in", [2, 3072])
    MODS_all = p.dram("MODS_all", [16, 3072])
    hT_d = p.dram("hT_d", [8, 128, T], BF16)
    QT_d = p.dram("QT_d", [8, 96, T], BF16)
    QdT_d = p.dram("QdT_d", [4, 128, T], BF16)
    Z_d = p.dram("Z_d", [T, 512])
    YF_d = p.dram("YF_d", [T, 512])
    YB_d = p.dram("YB_d", [T, 512])
    CT_d = p.dram("CT_d", [2, 128, T], BF16)
    EA_d = p.dram("EA_d", [T, 16])
    KTg = p.dram("KTg", [8 * 96, TL], BF16)
    KTc = p.dram("KTc", [8, 96, TC], BF16)
    Vg = p.dram("Vg", [8 * 128, NBL * 64], BF16)
    Vc = p.dram("Vc", [8, 128, 2 * 64], BF16)
    KdTg = p.dram("KdTg", [4 * 128, TL], BF16)
    KdTc = p.dram("KdTc", [4, 128, TC], BF16)
    Vdg = p.dram("Vdg", [4 * 128, NBL * 128], BF16)
    Vdc = p.dram("Vdc", [4, 128, 2 * 128], BF16)
    STg = p.dram("STg", [2 * 128, 512])
    STc = p.dram("STc", [2, 128, 512])
    DTg = p.dram("DTg", [2 * 128, 8])
    KT_all = p.dram("KT_all", [8 * 8 * 96, TL], BF16)
    V_all = p.dram("V_all", [8 * 8 * 128, NBL * 64], BF16)
    KdT_all = p.dram("KdT_all", [8 * 4 * 128, TL], BF16)
    Vd_all = p.dram("Vd_all", [8 * 4 * 128, NBL * 128], BF16)
    ST_all = p.dram("ST_all", [8 * 2 * 128, 512])
    DT_all = p.dram("DT_all", [8 * 2 * 128, 8])
    YAT = p.dram("YAT", [8, 64, T], BF16)
    YBT = p.dram("YBT", [4, 128, T], BF16)
    XMID = p.dram("XMID", [T, D])
    FT_in = p.dram("FT_in", [8 * 128, T], BF16)
    FT_all = p.dram("FT_all", [8 * 8 * 128, T], BF16)
    GT_in = p.dram("GT_in", [NEXP, T])
    GT_all = p.dram("GT_all", [8 * NEXP, T])
    PP = p.dram("PP", [8 * 8 * 128, TL])
    PS = p.dram("PS", [8 * 128, TL])
    PC = p.dram("PC", [8 * 128, TC])
    PCS = p.dram("PCS", [8 * 128, TC])

    cst = p.sb("cst", [128, 4, 128])
    p.dma(cst[:], consts_in.rearrange("c p q -> p c q"))
    ident_f, U_f, UT_f, ones_f = cst[:, 0, :], cst[:, 1, :], cst[:, 2, :], cst[:, 3, :]
    identb = p.sb("identb", [128, 128], BF16)
    p.copy(identb[:], ident_f)
    onesb = p.sb("onesb", [128, 128], BF16)
    p.copy(onesb[:], ones_f)
    misc = p.sb("misc", [128, 64])
    hmask = misc[:, 24:28]
    p.dma(hmask, hmask_in.partition_broadcast(128))
    cmask = misc[:, 32:48]
    p.dma(cmask, cmask_in.partition_broadcast(128))
    p.ts(misc[:, 48:64], cmask, -1.0, 1.0, ALU.mult, ALU.add)
    ncmask = misc[:, 48:64]
    halosel = p.sb("halosel", [32, 128])
    p.dma(halosel[:], halosel_in)
    sele = p.sb("sele", [NEXP, 4, 128])
    p.dma(sele[:], sele_in)
    F = [p.ps("pf%d" % i, [128, 512]) for i in range(7)]
    TBk = p.ps("pt0", [128, 1024], BF16)
    TB = [TBk, TBk]

    p.dma(XCUR[0:TC, :], ctx_in)
    p.dma(XCUR[TC:T, :], x_in)
    p.prefix = "M_"
    p.push()
    cc = p.sb("cc", [128, 8, 2])
    p.dma(cc[:], cc_in)
    scc = p.sb("scc", [128, 8, 2])
    p.act(scc[:], cc[:], AF.Silu)
    modrow = [p.sb("modrow%d" % i, [2, 512]) for i in range(2)]
    bmod2 = [p.sb("bmod2%d" % i, [2, 512]) for i in range(2)]
    wm = [p.sb("wm%d" % i, [128, 8, 512]) for i in range(2)]
    for n_ in range(6):
        w_ = wm[n_ % 2]
        cs = slice(n_ * 512, (n_ + 1) * 512)
        p.dma(w_[:], wmod_in[:, cs].rearrange("(k p) n -> p k n", p=128))
        p.dma(bmod2[n_ % 2][:], bmod_in[:, cs].partition_broadcast(2))
        for k in range(8):
            p.mm(F[n_ % 2][0:2, :], scc[:, k, :], w_[:, k, :], start=(k == 0), stop=(k == 7))
        p.tt(modrow[n_ % 2][:], F[n_ % 2][0:2, :], bmod2[n_ % 2][:], ALU.add)
        p.dma(MODS_in[:, cs], modrow[n_ % 2][:])
    p.cc("AllGather", ALU.bypass, MODS_in, MODS_all)
    p.pop()

    for l in range(depth):
        p.prefix = "L%d_" % l
        lat_last = (l == depth - 1)
        p.push()
        p.dma(HB_in[0:2, :], XCUR[TC:TC + 2, :])
        p.dma(HB_in[2:4, :], XCUR[T - 2:T, :])
        p.cc("AllGather", ALU.bypass, HB_in, HB_all)
        hb32 = p.sb("hb32", [32, D])
        p.dma(hb32[:], HB_all)
        halo_x = p.sb("halo_x", [128, D])
        for half in range(2):
            p.mm(F[half][:, :], halosel[:], hb32[:, half * 512:(half + 1) * 512])
            p.copy(halo_x[:, half * 512:(half + 1) * 512], F[half][:, :], eng="act")
        p.dma(XCUR[T:TH, :], halo_x[:])
        p.pop()

        p.push()
        small = p.sb("small", [128, 1024])
        p.dma(small[:], small_in[l:l + 1, :].partition_broadcast(128))
        gq_r, gkv_r = small[:, 0:256], small[:, 256:384]
        gqn_r, gkn_r = small[:, 384:480], small[:, 480:576]
        gdq_r, gdk_r = small[:, 576:640], small[:, 640:704]
        dtb_r, alog_r, dsk_r = small[:, 704:720], small[:, 720:736], small[:, 736:752]
        p.ts(gqn_r, gqn_r, 96.0 ** -0.5, None, ALU.mult)
        p.ts(gdq_r, gdq_r, 64.0 ** -0.5, None, ALU.mult)
        miscl = p.sb("miscl", [128, 32])
        a_rep = miscl[:, 0:16]
        p.act(a_rep, alog_r, AF.Exp)
        p.ts(a_rep, a_rep, -1.0, None, ALU.mult)
        dsum = miscl[:, 16:24]
        p.tt(dsum, dsk_r[:, 0:8], dsk_r[:, 8:16], ALU.add)
        convw = p.sb("convw", [128, 8, 5])
        convb = p.sb("convb", [128, 8])
        p.dma(convw[:], convw_in[l])
        p.dma(convb[:], convb_in[l])
        dt_all = p.sb("dt_all", [128, NT, 16])
        xpl = p.sb("xpl", [128, 8, TL + 4], BF16)
        xpc = p.sb("xpc", [128, 8, TC + 4], BF16)
        p.memset(xpc[:], 0.0)

        p.push()
        hT = p.sb("hT", [128, 8, TH], BF16)
        win = p.sb("win", [128, 8, GATE0], BF16)
        for k in range(8):
            for c0 in range(0, GATE0, 1752):
                p.dma(win[:, k, c0:c0 + 1752], win_in[l, k * 128:(k + 1) * 128, c0:c0 + 1752], q="pool")
        wuq = p.sb("wuq", [128, 2, 768], BF16)
        for k in range(2):
            p.dma(wuq[:, k, :], wuq_in[l, k * 128:(k + 1) * 128, :], q="pool")
        wukv = p.sb("wukv", [128, 1024], BF16)
        p.dma(wukv[:], wukv_in[l], q="pool")

        p.push()
        g1 = p.sb("g1", [128, D])
        p.dma(g1[:], g1_in[l:l + 1, :].partition_broadcast(128))
        modb = p.sb("modb", [128, 2, 2048])
        for r in range(2):
            p.dma(modb[:, r, :], MODS_all[4 * l + r:4 * l + r + 1, 0:2048].partition_broadcast(128))
        gs = p.sb("gs", [128, 2, D])
        for r in range(2):
            p.ts(gs[:, r, :], modb[:, r, 1024:2048], 1.0, None, ALU.add)
            p.tt(gs[:, r, :], gs[:, r, :], g1[:], ALU.mult)
        xt = [p.sb("xt%d" % i, [128, D]) for i in range(2)]
        junk = p.sb("junk", [128, D])
        h32 = p.sb("h32", [128, D])
        hb = [p.sb("hb%d" % i, [128, D], BF16) for i in range(2)]
        st1 = p.sb("st1", [128, 4])
        for i in range(NT + 1):
            x_ = xt[i % 2]
            p.dma(x_[:], XCUR[i * 128:(i + 1) * 128, :])
            p.act(junk[:], x_[:], AF.Square)
            p.reduce(st1[:, 0:1], junk[:])
            p.rstd(st1[:, 1:2], st1[:, 0:1], D)
            r = 1 if i < 2 else 0
            p.stt(h32[:], x_[:], st1[:, 1:2], gs[:, r, :], ALU.mult, ALU.mult)
            hb_ = hb[i % 2]
            p.tt(hb_[:], h32[:], modb[:, r, 0:1024], ALU.add)
            tb = TB[i % 2]
            for k in range(8):
                p.tr(tb[:, k * 128:(k + 1) * 128], hb_[:, k * 128:(k + 1) * 128], identb[:])
            p.copy(hT[:, :, i * 128:(i + 1) * 128], tb[:].rearrange("p (k t) -> p k t", k=8), eng="act")
        p.dma(hT_d.rearrange("k p t -> p k t"), hT[:, :, 0:T])
        p.pop()

        p.push()
        rope_t = p.sb("rope_t", [128, 192])
        sq = p.sb("sq", [128, 1024])
        st = p.sb("st", [128, 32])
        cqn = p.sb("cqn", [128, 384], BF16)
        cT = p.sb("cT", [128, 3, 128], BF16)
        qn = p.sb("qn", [128, 8, 96])
        kn = p.sb("kn", [128, 8, 96])
        qb = p.sb("qb", [128, 8, 96], BF16)
        kb = p.sb("kb", [128, 8, 96], BF16)
        vb = p.sb("vb", [128, 8, 64], BF16)
        rt1 = p.sb("rt1", [128, 8, 64])
        rt2 = p.sb("rt2", [128, 8, 64])
        qT_sb = p.sb("qT_sb", [128, 8, 128], BF16)
        kT_sb = p.sb("kT_sb", [128, 8, 128], BF16)
        dn = p.sb("dn", [128, 8, 64])
        db = p.sb("db", [128, 8, 64], BF16)
        dT_sb = p.sb("dT_sb", [128, 4, 128], BF16)
        vdb = p.sb("vdb", [128, 512], BF16)
        z32 = p.sb("z32", [128, 512])
        KTg_v = KTg.rearrange("(h p) t -> h p t", h=8)
        KdTg_v = KdTg.rearrange("(h p) t -> h p t", h=4)
        Vg_v = Vg.rearrange("(h p) (b d) -> h p b d", h=8, d=64)
        Vc_v = Vc.rearrange("h p (b d) -> h p b d", d=64)
        Vdg_v = Vdg.rearrange("(h p) (b d) -> h p b d", h=4, d=128)
        Vdc_v = Vdc.rearrange("h p (b d) -> h p b d", d=128)
        for i in range(NT):
            lat = i >= 2
            tok = slice(i * 128, (i + 1) * 128)
            ltok = slice((i - 2) * 128, (i - 1) * 128)
            if lat:
                p.dma(rope_t[:], rope_in[(i - 2) * 128:(i - 1) * 128, :])
            cos32, sin32, cos64, sin64 = rope_t[:, 0:32], rope_t[:, 32:64], rope_t[:, 64:128], rope_t[:, 128:192]

            def inproj(ps, c0, c1):
                for k in range(8):
                    p.mm(ps, hT[:, k, tok], win[:, k, c0:c1], start=(k == 0), stop=(k == 7))

            psM = F[0]
            inproj(psM[:, 0:416], 0, 416)
            inproj(psM[:, 416:432], DT0, DT0 + 16)
            p.act(sq[:, 0:416], psM[:, 0:416], AF.Square)
            p.reduce(st[:, 0:1], sq[:, 0:256])
            p.reduce(st[:, 1:2], sq[:, 256:384])
            p.reduce(st[:, 2:3], sq[:, 384:416])
            p.rstd(st[:, 3:4], st[:, 0:1], 256)
            p.rstd(st[:, 4:5], st[:, 1:2], 128)
            p.stt(cqn[:, 0:256], psM[:, 0:256], st[:, 3:4], gq_r, ALU.mult, ALU.mult)
            p.stt(cqn[:, 256:384], psM[:, 256:384], st[:, 4:5], gkv_r, ALU.mult, ALU.mult)
            p.tt(st[:, 16:32], psM[:, 416:432], dtb_r, ALU.add)
            p.act(st[:, 16:32], st[:, 16:32], AF.Exp)
            p.act(dt_all[:, i, :], st[:, 16:32], AF.Ln, bias=1.0)
            for c in range(3):
                p.tr(TB[0][:, c * 128:(c + 1) * 128], cqn[:, c * 128:(c + 1) * 128], identb[:])
            p.copy(cT[:], TB[0][:, 0:384].rearrange("p (c t) -> p c t", c=3), eng="act")
            for nh in range(2):
                for k in range(2):
                    p.mm(F[1 + nh][:, 0:384], cT[:, k, :], wuq[:, k, nh * 384:(nh + 1) * 384],
                         start=(k == 0), stop=(k == 1))
                p.mm(F[3 + nh][:, :], cT[:, 2, :], wukv[:, nh * 512:(nh + 1) * 512])
            for nh in range(2):
                p.act(sq[:, nh * 384:(nh + 1) * 384], F[1 + nh][:, 0:384], AF.Square)
            p.reduce(st[:, 8:16], sq[:, 0:768].rearrange("p (h d) -> p h d", h=8))
            p.rstd(st[:, 8:16], st[:, 8:16], 96)
            for nh in range(2):
                p.tt(qn[:, 4 * nh:4 * nh + 4, :], F[1 + nh][:, 0:384].rearrange("p (h d) -> p h d", h=4),
                     bc(st[:, 8 + 4 * nh:12 + 4 * nh].unsqueeze(2), [128, 4, 96]), ALU.mult)
            p.tt(qn[:], qn[:], bc(gqn_r.unsqueeze(1), [128, 8, 96]), ALU.mult)
            p.copy(qb[:, :, 0:64], qn[:, :, 0:64])
            if lat:
                rope_ops(p, qb[:, :, 64:96], qn[:, :, 64:96], cos32, sin32, 8, 8, rt1[:, :, 0:32], rt2[:, :, 0:32])
            else:
                p.copy(qb[:, :, 64:96], qn[:, :, 64:96])
            for h in range(8):
                p.tr(TB[1][0:96, h * 128:(h + 1) * 128], qb[:, h, :], identb[:])
            p.copy(qT_sb[0:96, :, :], TB[1][0:96, :].rearrange("p (h t) -> p h t", h=8), eng="act")
            p.dma(QT_d[:, :, tok].rearrange("h p t -> p h t"), qT_sb[0:96, :, :])
            for nh in range(2):
                p.act(sq[:, nh * 512:(nh + 1) * 512], F[3 + nh][:, :], AF.Square)
            p.reduce(st[:, 8:16], sq[:].rearrange("p (h d) -> p h d", h=8)[:, :, 0:64])
            p.tt(st[:, 8:16], st[:, 8:16], bc(st[:, 2:3], [128, 8]), ALU.add)
            p.rstd(st[:, 8:16], st[:, 8:16], 96)
            for nh in range(2):
                kv3 = F[3 + nh][:, :].rearrange("p (h d) -> p h d", h=4)
                p.tt(kn[:, 4 * nh:4 * nh + 4, 0:64], kv3[:, :, 0:64],
                     bc(st[:, 8 + 4 * nh:12 + 4 * nh].unsqueeze(2), [128, 4, 64]), ALU.mult)
                p.copy(vb[:, 4 * nh:4 * nh + 4, :], kv3[:, :, 64:128], eng="act")
            p.copy(sq[:, 0:32], psM[:, 384:416])
            p.tt(kn[:, :, 64:96], bc(sq[:, 0:32].unsqueeze(1), [128, 8, 32]),
                 bc(st[:, 8:16].unsqueeze(2), [128, 8, 32]), ALU.mult)
            p.tt(kn[:], kn[:], bc(gkn_r.unsqueeze(1), [128, 8, 96]), ALU.mult)
            p.copy(kb[:, :, 0:64], kn[:, :, 0:64])
            if lat:
                rope_ops(p, kb[:, :, 64:96], kn[:, :, 64:96], cos32, sin32, 8, 8, rt1[:, :, 0:32], rt2[:, :, 0:32])
            else:
                p.copy(kb[:, :, 64:96], kn[:, :, 64:96])
            for h in range(8):
                p.tr(TB[0][0:96, h * 128:(h + 1) * 128], kb[:, h, :], identb[:])
            p.copy(kT_sb[0:96, :, :], TB[0][0:96, :].rearrange("p (h t) -> p h t", h=8), eng="act")
            if lat:
                p.dma(KTg_v[:, :, ltok].rearrange("h p t -> p h t"), kT_sb[0:96, :, :])
                p.dma(Vg_v[:, :, i - 2, :].rearrange("h p d -> p h d"), vb[:])
            else:
                p.dma(KTc[:, :, tok].rearrange("h p t -> p h t"), kT_sb[0:96, :, :])
                p.dma(Vc_v[:, :, i, :].rearrange("h p d -> p h d"), vb[:])
            for which, c0, gam in (("q", DIF0, gdq_r), ("k", DIF0 + 512, gdk_r)):
                ps = F[5] if which == "q" else F[0]
                inproj(ps[:, :], c0, c0 + 512)
                p.act(sq[:, 0:512], ps[:, :], AF.Square)
                p.reduce(st[:, 8:16], sq[:, 0:512].rearrange("p (h d) -> p h d", h=8))
                p.rstd(st[:, 8:16], st[:, 8:16], 64)
                p.tt(dn[:], ps[:, :].rearrange("p (h d) -> p h d", h=8),
                     bc(st[:, 8:16].unsqueeze(2), [128, 8, 64]), ALU.mult)
                p.tt(dn[:], dn[:], bc(gam.unsqueeze(1), [128, 8, 64]), ALU.mult)
                if lat:
                    rope_ops(p, db[:], dn[:], cos64, sin64, 8, 16, rt1[:], rt2[:])
                else:
                    p.copy(db[:], dn[:])
                tb = TB[1]
                dbf = db[:].rearrange("p g d -> p (g d)")
                for h in range(4):
                    p.tr(tb[:, h * 128:(h + 1) * 128], dbf[:, h * 128:(h + 1) * 128], identb[:])
                p.copy(dT_sb[:], tb[:, 0:512].rearrange("p (h t) -> p h t", h=4), eng="act")
                if which == "q":
                    p.dma(QdT_d[:, :, tok].rearrange("h p t -> p h t"), dT_sb[:])
                elif lat:
                    p.dma(KdTg_v[:, :, ltok].rearrange("h p t -> p h t"), dT_sb[:])
                else:
                    p.dma(KdTc[:, :, tok].rearrange("h p t -> p h t"), dT_sb[:])
            inproj(F[1][:, :], DIF0 + 1024, DIF0 + 1536)
            p.copy(vdb[:], F[1][:, :], eng="act")
            vd3 = vdb[:].rearrange("p (h d) -> p h d", h=4)
            if lat:
                p.dma(Vdg_v[:, :, i - 2, :].rearrange("h p d -> p h d"), vd3)
            else:
                p.dma(Vdc_v[:, :, i, :].rearrange("h p d -> p h d"), vd3)
            inproj(F[2][:, :], Z0, Z0 + 512)
            p.copy(z32[:], F[2][:, :], eng="act")
            p.dma(Z_d[tok, :], z32[:])
        p.pop()
        p.cc("AllGather", ALU.bypass, KTg, KT_all)
        p.cc("AllGather", ALU.bypass, Vg, V_all)
        p.cc("AllGather", ALU.bypass, KdTg, KdT_all)
        p.cc("AllGather", ALU.bypass, Vdg, Vd_all)

        p.push()
        hal = p.sb("hal", [128, 4])
        for j in range(8):
            c0 = XBC0 + j * 128
            for k in range(8):
                p.mm(F[3][:, 0:TC], win[:, k, c0:c0 + 128], hT[:, k, 0:TC], start=(k == 0), stop=(k == 7))
            p.copy(xpc[:, j, 2:2 + TC], F[3][:, 0:TC], eng="act")
            for g0 in range(0, TL, 512):
                gl = min(512, TL - g0)
                ps = F[4 + (g0 // 512) % 2]
                for k in range(8):
                    p.mm(ps[:, 0:gl], win[:, k, c0:c0 + 128], hT[:, k, TC + g0:TC + g0 + gl],
                         start=(k == 0), stop=(k == 7))
                p.copy(xpl[:, j, 2 + g0:2 + g0 + gl], ps[:, 0:gl], eng="act")
            for k in range(8):
                p.mm(F[3][:, 256:260], win[:, k, c0:c0 + 128], hT[:, k, T:T + 4], start=(k == 0), stop=(k == 7))
            p.tt(hal[:], F[3][:, 256:260], hmask, ALU.mult)
            p.copy(xpl[:, j, 0:2], hal[:, 0:2])
            p.copy(xpl[:, j, TL + 2:TL + 4], hal[:, 2:4])
        p.pop()
        p.pop()

        p.push()
        BT = p.sb("BT", [128, 2, T], BF16)
        CT = p.sb("CT", [128, 2, T], BF16)
        xs_tok = p.sb("xs_tok", [128, NT, 512], BF16)
        Btok = p.sb("Btok", [128, NT, 256], BF16)
        cv = p.sb("cv", [128, 512])
        cvb = p.sb("cvb", [128, 512], BF16)
        segs = [(xpc, 0, TC), (xpl, TC, TL)]
        for j in range(8):
            for (xp, t0, tl) in segs:
                for g0 in range(0, tl, 512):
                    gl = min(512, tl - g0)
                    p.ts(cv[:, 0:gl], xp[:, j, g0:g0 + gl], convw[:, j, 0:1], convb[:, j:j + 1], ALU.mult, ALU.add)
                    for kk in range(1, 5):
                        p.stt(cv[:, 0:gl], xp[:, j, g0 + kk:g0 + kk + gl], convw[:, j, kk:kk + 1], cv[:, 0:gl],
                              ALU.mult, ALU.add)
                    a0 = t0 + g0
                    if j < 6:
                        dst = cvb[:, 0:gl] if j < 4 else BT[:, j - 4, a0:a0 + gl]
                        p.act(dst, cv[:, 0:gl], AF.Silu)
                        tb = TB[(g0 // 512) % 2]
                        for tt_ in range(gl // 128):
                            p.tr(tb[:, tt_ * 128:(tt_ + 1) * 128], dst[:, tt_ * 128:(tt_ + 1) * 128], identb[:])
                        ti0 = a0 // 128
                        if j < 4:
                            p.copy(xs_tok[:, ti0:ti0 + gl // 128, j * 128:(j + 1) * 128],
                                   tb[:, 0:gl].rearrange("p (t c) -> p t c", c=128), eng="act")
                        else:
                            p.copy(Btok[:, ti0:ti0 + gl // 128, (j - 4) * 128:(j - 3) * 128],
                                   tb[:, 0:gl].rearrange("p (t c) -> p t c", c=128), eng="act")
                    else:
                        p.act(CT[:, j - 6, a0:a0 + gl], cv[:, 0:gl], AF.Silu)
        p.dma(CT_d.rearrange("g p t -> p g t"), CT[:])
        H = p.sb("H", [128, 512])
        Hb = p.sb("Hb", [128, 512], BF16)
        base = p.sb("base", [128, 8])
        dtA = p.sb("dtA", [128, 8])
        acum = p.sb("acum", [128, 16])
        Ms = p.sb("Ms", [128, 8, 128])
        dif = p.sb("dif", [128, 8, 128])
        seg = p.sb("seg", [128, 8, 128])
        cb = p.sb("cb", [128, 2, 128])
        scT = p.sb("scT", [128, 8, 128], BF16)
        xd = p.sb("xd", [128, 8, 64], BF16)
        xdd = p.sb("xdd", [128, 8, 64], BF16)
        t8 = p.sb("t8", [128, 32])
        yo = p.sb("yo", [128, 8, 64])
        yy = p.sb("yy", [128, 8, 64])
        eaB = p.sb("eaB", [128, 8])
        STg_v = STg.rearrange("(d p) n -> d p n", d=2)
        DTg_v = DTg.rearrange("(d p) n -> d p n", d=2)
        for (seg_i, tiles) in ((1, list(range(2, NT))), (0, [0, 1])):
            for d in range(2):
                order = tiles if d == 0 else tiles[::-1]
                Md = U_f if d == 0 else UT_f
                p.memset(H[:], 0.0)
                p.memset(Hb[:], 0.0)
                p.memset(base[:], 0.0)
                for i in order:
                    tok = slice(i * 128, (i + 1) * 128)
                    dt = dt_all[:, i, d * 8:(d + 1) * 8]
                    p.tt(dtA[:], dt, a_rep[:, d * 8:(d + 1) * 8], ALU.mult)
                    p.mm(F[0][:, 0:8], Md, dtA[:])
                    p.mm(F[0][:, 8:16], ones_f, dtA[:])
                    p.copy(acum[:], F[0][:, 0:16])
                    tot = acum[:, 8:16]
                    p.tt(Ms[:], bc(Md.unsqueeze(1), [128, 8, 128]), bc(dtA[:].unsqueeze(2), [128, 8, 128]), ALU.mult)
                    Msf = Ms[:].rearrange("p h l -> p (h l)")
                    for hh in range(2):
                        p.mm(F[1 + hh][:, :], ones_f, Msf[:, hh * 512:(hh + 1) * 512])
                        p.tt(dif[:, 4 * hh:4 * hh + 4, :], F[1 + hh][:, :].rearrange("p (h l) -> p h l", h=4),
                             bc(acum[:, 4 * hh:4 * hh + 4].unsqueeze(2), [128, 4, 128]), ALU.subtract)
                    p.act(dif[:], dif[:], AF.Exp)
                    p.stt(seg[:], dif[:], 1.0, bc(Md.unsqueeze(1), [128, 8, 128]), ALU.min, ALU.mult)
                    for g in range(2):
                        p.mm(F[3][:, g * 128:(g + 1) * 128], BT[:, g, tok], CT[:, g, tok])
                    p.copy(cb[:], F[3][:, 0:256].rearrange("p (g l) -> p g l", g=2), eng="act")
                    for g in range(2):
                        p.tt(scT[:, 4 * g:4 * g + 4, :], seg[:, 4 * g:4 * g + 4, :],
                             bc(cb[:, g, :].unsqueeze(1), [128, 4, 128]), ALU.mult)
                    xs3 = xs_tok[:, i, :].rearrange("p (h d) -> p h d", h=8)
                    p.tt(xd[:], xs3, bc(dt.unsqueeze(2), [128, 8, 64]), ALU.mult)
                    for h in range(8):
                        p.mm(F[4][:, h * 64:(h + 1) * 64], scT[:, h, :], xd[:, h, :])
                    p.tt(t8[:, 0:8], tot, acum[:, 0:8], ALU.subtract)
                    p.act(t8[:, 0:8], t8[:, 0:8], AF.Exp)
                    p.tt(xdd[:], xd[:], bc(t8[:, 0:8].unsqueeze(2), [128, 8, 64]), ALU.mult)
                    xddf = xdd[:].rearrange("p h d -> p (h d)")
                    for g in range(2):
                        p.mm(F[5][:, g * 256:(g + 1) * 256], Btok[:, i, g * 128:(g + 1) * 128],
                             xddf[:, g * 256:(g + 1) * 256])
                    for g in range(2):
                        p.mm(F[3][:, g * 256:(g + 1) * 256], CT[:, g, tok], Hb[:, g * 256:(g + 1) * 256])
                    p.act(t8[:, 8:16], acum[:, 0:8], AF.Exp)
                    p.tt(yo[:], F[3][:, :].rearrange("p (h d) -> p h d", h=8),
                         bc(t8[:, 8:16].unsqueeze(2), [128, 8, 64]), ALU.mult)
                    p.tt(yy[:], F[4][:, :].rearrange("p (h d) -> p h d", h=8), yo[:], ALU.add)
                    if d == 0:
                        p.tt(yo[:], xs3, bc(dsum.unsqueeze(2), [128, 8, 64]), ALU.mult)
                        p.tt(yy[:], yy[:], yo[:], ALU.add)
                    p.dma((YF_d if d == 0 else YB_d)[tok, :], yy[:].rearrange("p h d -> p (h d)"))
                    p.tt(t8[:, 16:24], acum[:, 0:8], base[:], ALU.add)
                    p.act(eaB[:], t8[:, 16:24], AF.Exp)
                    p.dma(EA_d[tok, d * 8:(d + 1) * 8], eaB[:])
                    p.act(t8[:, 24:32], tot, AF.Exp)
                    H3 = H[:].rearrange("p (h d) -> p h d", h=8)
                    p.tt(H3, H3, bc(t8[:, 24:32].unsqueeze(2), [128, 8, 64]), ALU.mult)
                    p.tt(H[:], H[:], F[5][:, :], ALU.add)
                    p.copy(Hb[:], H[:], eng="act")
                    p.tt(base[:], base[:], tot, ALU.add)
                if seg_i == 1:
                    p.dma(STg_v[d], H[:])
                    p.act(eaB[:], base[:], AF.Exp)
                    p.dma(DTg_v[d], eaB[:])
                else:
                    p.dma(STc[d], H[:])
        p.pop()
        p.pop()
        p.cc("AllGather", ALU.bypass, STg, ST_all)
        p.cc("AllGather", ALU.bypass, DTg, DT_all)

        KT_all_v = KT_all.rearrange("(r h p) t -> r h p t", r=8, h=8)
        V_all_v = V_all.rearrange("(r h p) (b d) -> r h p b d", r=8, h=8, d=64)
        KdT_all_v = KdT_all.rearrange("(r h p) t -> r h p t", r=8, h=4)
        Vd_all_v = Vd_all.rearrange("(r h p) (b d) -> r h p b d", r=8, h=4, d=128)
        p.push()
        KTb = [p.sb("KTb%d" % i, [96, NK], BF16) for i in range(2)]
        Vb = [p.sb("Vb%d" % i, [128, NKB, 65], BF16) for i in range(2)]
        Qh = [p.sb("Qh%d" % i, [96, T], BF16) for i in range(2)]
        PT = [p.sb("PT%d" % i, [128, 512], BF16) for i in range(3)]
        rl = p.sb("rl", [128, 512])
        bcs = p.sb("bcs", [64, 512])
        yat = [p.sb("yat%d" % i, [64, 512], BF16) for i in range(2)]
        for i in range(2):
            p.memset(Vb[i][:], 1.0)
        cnt = 0
        gcnt = 0
        for h in range(8):
            kt, vv, qh = KTb[h % 2], Vb[h % 2], Qh[h % 2]
            p.dma(kt[:, 0:S].rearrange("p (r t) -> p r t", r=8), KT_all_v[:, h, :, :].rearrange("r p t -> p r t"))
            p.dma(kt[:, S:NK], KTc[h])
            for r in range(8):
                p.dma(vv[:, r * NBL:(r + 1) * NBL, 0:64], V_all_v[r, h])
            p.dma(vv[:, NKB - 2:NKB, 0:64], Vc_v[h])
            p.dma(qh[:], QT_d[h])
            for (q0, gl, is_ctx) in groups:
                kbs = list(range(NKB - 2, NKB)) if is_ctx else list(range(NKB))
                pso = F[3 + gcnt % 2]
                for n_, kb_ in enumerate(kbs):
                    pss = F[cnt % 3]
                    pt = PT[cnt % 3]
                    cnt += 1
                    p.mm(pss[:, 0:gl], kt[:, kb_ * 128:(kb_ + 1) * 128], qh[:, q0:q0 + gl])
                    p.act(pt[:, 0:gl], pss[:, 0:gl], AF.Exp)
                    p.mm(pso[0:65, 0:gl], vv[:, kb_, :], pt[:, 0:gl], start=(n_ == 0), stop=(n_ == len(kbs) - 1))
                p.recip(rl[64:65, 0:gl], pso[64:65, 0:gl])
                p.mm(F[5][0:64, 0:gl], ones_f[64:65, 0:64], rl[64:65, 0:gl])
                p.copy(bcs[:, 0:gl], F[5][0:64, 0:gl], eng="act")
                ya = yat[gcnt % 2]
                p.tt(ya[:, 0:gl], pso[0:64, 0:gl], bcs[:, 0:gl], ALU.mult)
                p.dma(YAT[h, :, q0:q0 + gl], ya[:, 0:gl])
                gcnt += 1
        p.pop()

        p.push()
        lamt = p.sb("lamt", [128, 8])
        dl = p.sb("dl", [128, 256])
        p.dma(dl[:], dlam_in[l:l + 1, :].partition_broadcast(128))
        p.dma(lamt[:, 0:2], lam_in[l:l + 1, :].partition_broadcast(128))
        gsub = p.sb("gsub", [128, 1])
        p.dma(gsub[:], gsub_in[l])
        p.ts(gsub[:], gsub[:], lamt[:, 1:2], None, ALU.mult)
        p.tt(dl[:, 0:64], dl[:, 0:64], dl[:, 64:128], ALU.mult)
        p.tt(dl[:, 128:192], dl[:, 128:192], dl[:, 192:256], ALU.mult)
        p.reduce(lamt[:, 2:3], dl[:, 0:64])
        p.reduce(lamt[:, 3:4], dl[:, 128:192])
        p.act(lamt[:, 2:4], lamt[:, 2:4], AF.Exp)
        p.tt(lamt[:, 4:5], lamt[:, 2:3], lamt[:, 3:4], ALU.subtract)
        p.tt(lamt[:, 4:5], lamt[:, 4:5], lamt[:, 0:1], ALU.add)
        p.ts(lamt[:, 5:6], lamt[:, 4:5], -1.0, None, ALU.mult)
        KTd = [p.sb("KTd%d" % i, [128, NK], BF16) for i in range(2)]
        Vd = [p.sb("Vd%d" % i, [128, NKB, 128], BF16) for i in range(2)]
        Qd = [p.sb("Qd%d" % i, [128, T], BF16) for i in range(2)]
        PT = [p.sb("PTd%d" % i, [128, 512], BF16) for i in range(4)]
        rl = p.sb("rld", [128, 512])
        b1 = p.sb("b1", [128, 512])
        b2 = p.sb("b2", [128, 512])
        o1 = p.sb("o1", [128, 512])
        o2 = p.sb("o2", [128, 512])
        ybt = [p.sb("ybt%d" % i, [128, 512], BF16) for i in range(2)]
        cnt = 0
        gcnt = 0
        for h in range(4):
            kt, vv, qh = KTd[h % 2], Vd[h % 2], Qd[h % 2]
            p.dma(kt[:, 0:S].rearrange("p (r t) -> p r t", r=8), KdT_all_v[:, h, :, :].rearrange("r p t -> p r t"))
            p.dma(kt[:, S:NK], KdTc[h])
            for r in range(8):
                p.dma(vv[:, r * NBL:(r + 1) * NBL, :], Vd_all_v[r, h])
            p.dma(vv[:, NKB - 2:NKB, :], Vdc_v[h])
            p.dma(qh[:], QdT_d[h])
            for (q0, gl, is_ctx) in groups:
                kbs = list(range(NKB - 2, NKB)) if is_ctx else list(range(NKB))
                for n_, kb_ in enumerate(kbs):
                    st_, sp_ = (n_ == 0), (n_ == len(kbs) - 1)
                    for m in range(2):
                        pss = F[(cnt % 2) * 2 + m]
                        pt = PT[(cnt % 2) * 2 + m]
                        ks = slice(m * 64, (m + 1) * 64)
                        p.mm(pss[:, 0:gl], kt[ks, kb_ * 128:(kb_ + 1) * 128], qh[ks, q0:q0 + gl])
                        p.act(pt[:, 0:gl], pss[:, 0:gl], AF.Exp)
                        p.mm(F[4 + m][:, 0:gl], vv[:, kb_, :], pt[:, 0:gl], start=st_, stop=sp_)
                        p.mm(F[6][32 * m:32 * m + 1, 0:gl], onesb[:, 0:1], pt[:, 0:gl], start=st_, stop=sp_)
                    cnt += 1
                for m in range(2):
                    p.recip(rl[32 * m:32 * m + 1, 0:gl], F[6][32 * m:32 * m + 1, 0:gl])
                    p.mm(F[m][:, 0:gl], ones_f[32 * m:32 * m + 1, :], rl[32 * m:32 * m + 1, 0:gl])
                p.copy(b1[:, 0:gl], F[0][:, 0:gl], eng="act")
                p.copy(b2[:, 0:gl], F[1][:, 0:gl], eng="act")
                p.tt(o1[:, 0:gl], F[4][:, 0:gl], b1[:, 0:gl], ALU.mult)
                p.tt(o2[:, 0:gl], F[5][:, 0:gl], b2[:, 0:gl], ALU.mult)
                p.stt(o1[:, 0:gl], o2[:, 0:gl], lamt[:, 5:6], o1[:, 0:gl], ALU.mult, ALU.add)
                p.act(o2[:, 0:gl], o1[:, 0:gl], AF.Square)
                p.mm(F[2][:, 0:gl], ones_f, o2[:, 0:gl])
                p.act(b1[:, 0:gl], F[2][:, 0:gl], AF.Sqrt, bias=EPS, scale=1.0 / 128)
                p.recip(b1[:, 0:gl], b1[:, 0:gl])
                yb = ybt[gcnt % 2]
                p.stt(yb[:, 0:gl], o1[:, 0:gl], gsub[:, 0:1], b1[:, 0:gl], ALU.mult, ALU.mult)
                p.dma(YBT[h, :, q0:q0 + gl], yb[:, 0:gl])
                gcnt += 1
        p.pop()

        p.push()
        modb = p.sb("modb3", [128, 2, 3 * D])
        m32 = p.sb("m32", [128, D])
        for r in range(2):
            p.dma(modb[:, r, 0:1024], MODS_all[4 * l + r:4 * l + r + 1, 2048:3072].partition_broadcast(128))
            p.dma(modb[:, r, 1024:3072], MODS_all[4 * l + 2 + r:4 * l + 3 + r, 0:2048].partition_broadcast(128))
        p.dma(m32[:], g2_in[l:l + 1, :].partition_broadcast(128))
        for r in range(2):
            p.ts(modb[:, r, 2048:3072], modb[:, r, 2048:3072], 1.0, None, ALU.add)
            p.tt(modb[:, r, 2048:3072], modb[:, r, 2048:3072], m32[:], ALU.mult)
        gssm = p.sb("gssm", [128, 512])
        p.dma(gssm[:], gssm_in[l:l + 1, :].partition_broadcast(128))
        bgate = p.sb("bgate", [128, 3072])
        p.dma(bgate[:], bgate_in[l:l + 1, :].partition_broadcast(128))
        brt = p.sb("brt", [128, NEXP])
        p.dma(brt[:], br_in[l:l + 1, :].partition_broadcast(128))
        wg = p.sb("wg", [128, 8, 3072], BF16)
        for k in range(8):
            for c0 in range(0, 3072, 1536):
                p.dma(wg[:, k, c0:c0 + 1536], win_in[l, k * 128:(k + 1) * 128, GATE0 + c0:GATE0 + c0 + 1536], q="pool")
        wupa = p.sb("wupa", [64, 8, D], BF16)
        p.dma(wupa[:], wupa_in[l].rearrange("(h p) n -> p h n", p=64), q="pool")
        wupd = p.sb("wupd", [128, 4, D], BF16)
        p.dma(wupd[:], wupd_in[l].rearrange("(h p) n -> p h n", p=128), q="pool")
        wups = p.sb("wups", [128, 4, D], BF16)
        p.dma(wups[:], wups_in[l].rearrange("(h p) n -> p h n", p=128), q="pool")
        wout = p.sb("wout", [128, 8, D], BF16)
        for k in range(8):
            p.dma(wout[:, k, :], wout_in[l, k * 128:(k + 1) * 128, :], q="pool")
        wr = p.sb("wr", [128, 8, NEXP])
        p.dma(wr[:], wr_in[l].rearrange("(k p) e -> p k e", p=128))
        Hin = p.sb("Hin", [128, 2, 512])
        Hinb = p.sb("Hinb", [128, 2, 512], BF16)
        sch = p.sb("sch", [128, 512])
        dch = p.sb("dch", [128, 8])
        ST_all_v = ST_all.rearrange("(r d p) n -> r d p n", r=8, d=2)
        DT_all_v = DT_all.rearrange("(r d p) n -> r d p n", r=8, d=2)
        for d in range(2):
            p.dma(Hin[:, d, :], STc[d])
            order = list(range(8)) if d == 0 else list(range(7, -1, -1))
            for j in order:
                mcol = cmask[:, d * 8 + j:d * 8 + j + 1]
                ncol = ncmask[:, d * 8 + j:d * 8 + j + 1]
                p.dma(sch[:], ST_all_v[j, d])
                p.dma(dch[:], DT_all_v[j, d])
                p.ts(dch[:], dch[:], mcol, ncol, ALU.mult, ALU.add)
                p.ts(sch[:], sch[:], mcol, None, ALU.mult)
                H3 = Hin[:, d, :].rearrange("p (h d) -> p h d", h=8)
                p.tt(H3, H3, bc(dch[:].unsqueeze(2), [128, 8, 64]), ALU.mult)
                p.tt(Hin[:, d, :], Hin[:, d, :], sch[:], ALU.add)
            p.copy(Hinb[:, d, :], Hin[:, d, :])
        yf = p.sb("yf", [128, 512])
        ybk = p.sb("ybk", [128, 512])
        zt = p.sb("zt", [128, 512])
        ea = p.sb("ea", [128, 16])
        ctt = p.sb("ctt", [128, 2, 128], BF16)
        tmp = p.sb("tmp", [128, 512])
        st = p.sb("st3", [128, 16])
        ycb = p.sb("ycb", [128, 512], BF16)
        ycT = p.sb("ycT", [128, 4, 128], BF16)
        yaTt = p.sb("yaTt", [64, 8, 128], BF16)
        ybTt = p.sb("ybTt", [128, 4, 128], BF16)
        hTt = p.sb("hTt", [128, 8, 128], BF16)
        gt_ = p.sb("gt_", [128, 512])
        mb = p.sb("mb", [128, D], BF16)
        mT = p.sb("mT", [128, 8, 128], BF16)
        xt = p.sb("xt3", [128, D])
        xm = p.sb("xm", [128, D])
        junk = m32
        f32t = m32
        fT32 = p.sb("fT32", [128, 8, 128])
        fTb = p.sb("fTb", [128, 8, 128], BF16)
        lg = p.sb("lg", [128, NEXP])
        eg = p.sb("eg", [128, NEXP])
        m8 = p.sb("m8", [128, 8])
        gT_sb = p.sb("gT_sb", [NEXP, 128])
        FT_in_v = FT_in.rearrange("(k p) t -> k p t", k=8)
        for i in range(NT):
            lat = i >= 2
            r = 0 if lat else 1
            tok = slice(i * 128, (i + 1) * 128)
            p.dma(yf[:], YF_d[tok, :])
            p.dma(ybk[:], YB_d[tok, :])
            p.dma(zt[:], Z_d[tok, :])
            p.tt(yf[:], yf[:], ybk[:], ALU.add)
            if lat:
                p.dma(ea[:], EA_d[tok, :])
                p.dma(ctt[:], CT_d[:, :, tok].rearrange("g p t -> p g t"))
                for d in range(2):
                    for g in range(2):
                        p.mm(F[0][:, g * 256:(g + 1) * 256], ctt[:, g, :], Hinb[:, d, g * 256:(g + 1) * 256])
                    p.tt(tmp[:].rearrange("p (h d) -> p h d", h=8), F[0][:, :].rearrange("p (h d) -> p h d", h=8),
                         bc(ea[:, d * 8:(d + 1) * 8].unsqueeze(2), [128, 8, 64]), ALU.mult)
                    p.tt(yf[:], yf[:], tmp[:], ALU.add)
            p.act(tmp[:], zt[:], AF.Silu)
            p.tt(yf[:], yf[:], tmp[:], ALU.mult)
            p.act(tmp[:], yf[:], AF.Square)
            p.reduce(st[:, 0:2], tmp[:].rearrange("p (g d) -> p g d", g=2))
            p.rstd(st[:, 2:4], st[:, 0:2], 256)
            p.tt(yf[:].rearrange("p (g d) -> p g d", g=2), yf[:].rearrange("p (g d) -> p g d", g=2),
                 bc(st[:, 2:4].unsqueeze(2), [128, 2, 256]), ALU.mult)
            p.tt(ycb[:], yf[:], gssm[:], ALU.mult)
            for c in range(4):
                p.tr(TBk[:, c * 128:(c + 1) * 128], ycb[:, c * 128:(c + 1) * 128], identb[:])
            p.copy(ycT[:], TBk[:, 0:512].rearrange("p (c t) -> p c t", c=4), eng="act")
            p.dma(yaTt[:], YAT[:, :, tok].rearrange("h p t -> p h t"))
            p.dma(ybTt[:], YBT[:, :, tok].rearrange("h p t -> p h t"))
            p.dma(hTt[:], hT_d[:, :, tok].rearrange("k p t -> p k t"))
            for half in range(2):
                cs = slice(half * 512, (half + 1) * 512)
                for h in range(8):
                    p.mm(F[0][:, :], yaTt[:, h, :], wupa[:, h, cs], start=(h == 0), stop=(h == 7))
                for h in range(4):
                    p.mm(F[1][:, :], ybTt[:, h, :], wupd[:, h, cs], start=(h == 0), stop=(h == 3))
                for h in range(4):
                    p.mm(F[2][:, :], ycT[:, h, :], wups[:, h, cs], start=(h == 0), stop=(h == 3))
                for b_ in range(3):
                    gc = slice(b_ * 1024 + half * 512, b_ * 1024 + (half + 1) * 512)
                    for k in range(8):
                        p.mm(F[3 + b_][:, :], hTt[:, k, :], wg[:, k, gc], start=(k == 0), stop=(k == 7))
                    p.tt(gt_[:], F[3 + b_][:, :], bgate[:, gc], ALU.add)
                    p.act(gt_[:], gt_[:], AF.Sigmoid)
                    if b_ == 0:
                        p.tt(m32[:, cs], gt_[:], F[b_][:, :], ALU.mult)
                    else:
                        p.tt(gt_[:], gt_[:], F[b_][:, :], ALU.mult)
                        p.tt(m32[:, cs], m32[:, cs], gt_[:], ALU.add)
            p.copy(mb[:], m32[:])
            for k in range(8):
                p.tr(TBk[:, k * 128:(k + 1) * 128], mb[:, k * 128:(k + 1) * 128], identb[:])
            p.copy(mT[:], TBk[:].rearrange("p (k t) -> p k t", k=8), eng="act")
            p.dma(xt[:], XCUR[tok, :])
            for half in range(2):
                cs = slice(half * 512, (half + 1) * 512)
                for k in range(8):
                    p.mm(F[5 + half][:, :], mT[:, k, :], wout[:, k, cs], start=(k == 0), stop=(k == 7))
                p.tt(xm[:, cs], F[5 + half][:, :], modb[:, r, 0:1024][:, cs], ALU.mult)
            p.tt(xm[:], xm[:], xt[:], ALU.add)
            p.dma(XMID[tok, :], xm[:])
            p.act(junk[:], xm[:], AF.Square)
            p.reduce(st[:, 4:5], junk[:])
            p.rstd(st[:, 5:6], st[:, 4:5], D)
            p.stt(f32t[:], xm[:], st[:, 5:6], modb[:, r, 2048:3072], ALU.mult, ALU.mult)
            p.tt(f32t[:], f32t[:], modb[:, r, 1024:2048], ALU.add)
            for half in range(2):
                for k in range(4):
                    kk = half * 4 + k
                    p.tr(F[half][:, k * 128:(k + 1) * 128], f32t[:, kk * 128:(kk + 1) * 128], ident_f)
                p.copy(fT32[:, half * 4:half * 4 + 4, :], F[half][:, :].rearrange("p (k t) -> p k t", k=4), eng="act")
            p.copy(fTb[:], fT32[:])
            p.dma(FT_in_v[:, :, tok].rearrange("k p t -> p k t"), fTb[:])
            for k in range(8):
                p.mm(F[2][:, 0:NEXP], fT32[:, k, :], wr[:, k, :], start=(k == 0), stop=(k == 7))
            p.tt(lg[:], F[2][:, 0:NEXP], brt[:], ALU.add)
            p.add("dve", lambda e, o=m8, i_=lg: e.max(o[:], i_[:]), [lg[:]], [m8[:]])
            p.ts(st[:, 6:7], m8[:, 0:1], -1.0, None, ALU.mult)
            p.act(eg[:], lg[:], AF.Exp, bias=st[:, 6:7])
            p.stt(eg[:], lg[:], m8[:, 3:4], eg[:], ALU.is_ge, ALU.mult)
            p.reduce(st[:, 7:8], eg[:])
            p.recip(st[:, 7:8], st[:, 7:8])
            p.ts(eg[:], eg[:], st[:, 7:8], None, ALU.mult)
            p.tr(F[3][0:NEXP, 0:128], eg[:], ident_f)
            p.copy(gT_sb[:], F[3][0:NEXP, 0:128], eng="act")
            p.dma(GT_in[:, tok], gT_sb[:])
        p.pop()
        p.cc("AllGather", ALU.bypass, FT_in, FT_all)
        p.cc("AllGather", ALU.bypass, GT_in, GT_all)

        FT_all_v = FT_all.rearrange("(r k p) t -> r k p t", r=8, k=8)
        GT_all_v = GT_all.rearrange("(r e) t -> r e t", r=8)
        PP_v = PP.rearrange("(r k p) t -> r k p t", r=8, k=8)
        PC_v = PC.rearrange("(k p) t -> k p t", k=8)
        p.push()
        bgu = p.sb("bgu", [128, 4, 16])
        p.dma(bgu[:], bgu_in[l])
        bdn = p.sb("bdn", [NEXP, D])
        p.dma(bdn[:], bdn_in[l])
        fTk = p.sb("fTk", [128, 8, 2048], BF16)
        gTk = p.sb("gTk", [NEXP, 2048])
        acc = p.sb("acc", [128, 8, 2048])
        wgu = [p.sb("wgu%d" % i, [128, 8, 1024], BF16) for i in range(2)]
        wdn = [p.sb("wdn%d" % i, [128, 4, D], BF16) for i in range(2)]
        Gb = p.sb("Gb", [128, 512])
        gg = [p.sb("gg%d" % i, [128, 512]) for i in range(2)]
        ss = [p.sb("ss%d" % i, [128, 512]) for i in range(2)]
        tl_ = [p.sb("tl%d" % i, [128, 512]) for i in range(2)]
        actT = [p.sb("actT%d" % i, [128, 4, 512], BF16) for i in range(2)]
        ucnt = 0
        jc = 0
        dcn = 0
        blocks = [(r, TC, TL) for r in range(8)] + [(0, 0, TC)]
        for bi, (rr, c0_, bl) in enumerate(blocks):
            is_cb = (bi == 8)
            p.dma(fTk[:, :, 0:bl], FT_all_v[rr, :, :, c0_:c0_ + bl].rearrange("k p t -> p k t"))
            p.dma(gTk[:, 0:bl], GT_all_v[rr, :, c0_:c0_ + bl])
            tgs = [(g0, min(512, bl - g0)) for g0 in range(0, bl, 512)]
            for (q0, gl) in tgs:
                for dc in range(8):
                    ps = F[dc % 2]
                    p.mm(ps[:, 0:gl], bdn[:, dc * 128:(dc + 1) * 128], gTk[:, q0:q0 + gl])
                    p.copy(acc[:, dc, q0:q0 + gl], ps[:, 0:gl], eng="act")
            for e_ in range(4):
                for half in range(2):
                    wg_, wd_ = wgu[ucnt % 2], wdn[ucnt % 2]
                    for k in range(8):
                        rows = slice(k * 128, (k + 1) * 128)
                        p.dma(wg_[:, k, 0:512], wgu_in[l, e_, rows, half * 512:(half + 1) * 512], q="pool")
                        p.dma(wg_[:, k, 512:1024], wgu_in[l, e_, rows, DFF + half * 512:DFF + (half + 1) * 512], q="pool")
                    for c in range(4):
                        for cc0 in range(0, D, 512):
                            p.dma(wd_[:, c, cc0:cc0 + 512],
                                  wdn_in[l, e_, half * 512 + c * 128:half * 512 + (c + 1) * 128, cc0:cc0 + 512], q="pool")
                    for gi, (q0, gl) in enumerate(tgs):
                        aT = actT[gi % 2]
                        p.mm(F[6][:, 0:gl], sele[:, e_, :], gTk[:, q0:q0 + gl])
                        p.copy(Gb[:, 0:gl], F[6][:, 0:gl], eng="act")
                        for jj in range(4):
                            j = half * 4 + jj
                            psg, psl = F[(jc % 2) * 2], F[(jc % 2) * 2 + 1]
                            g_, s_, t_ = gg[jc % 2], ss[jc % 2], tl_[jc % 2]
                            jc += 1
                            for k in range(8):
                                p.mm(psg[:, 0:gl], wg_[:, k, jj * 128:(jj + 1) * 128], fTk[:, k, q0:q0 + gl],
                                     start=(k == 0), stop=(k == 7))
                            for k in range(8):
                                p.mm(psl[:, 0:gl], wg_[:, k, 512 + jj * 128:512 + (jj + 1) * 128], fTk[:, k, q0:q0 + gl],
                                     start=(k == 0), stop=(k == 7))
                            p.ts(g_[:, 0:gl], psg[:, 0:gl], bgu[:, e_, j:j + 1], 7.0, ALU.add, ALU.min)
                            p.act(s_[:, 0:gl], g_[:, 0:gl], AF.Sigmoid, scale=1.702)
                            p.ts(t_[:, 0:gl], psl[:, 0:gl], bgu[:, e_, 8 + j:9 + j], 7.0, ALU.add, ALU.min)
                            p.ts(t_[:, 0:gl], t_[:, 0:gl], -7.0, 1.0, ALU.max, ALU.add, eng="pool")
                            p.tt(g_[:, 0:gl], g_[:, 0:gl], s_[:, 0:gl], ALU.mult, eng="pool")
                            p.tt(g_[:, 0:gl], g_[:, 0:gl], t_[:, 0:gl], ALU.mult, eng="pool")
                            p.tt(aT[:, jj, 0:gl], g_[:, 0:gl], Gb[:, 0:gl], ALU.mult)
                        for dc in range(8):
                            psd = F[4 + dcn % 2]
                            dcn += 1
                            for jj in range(4):
                                p.mm(psd[:, 0:gl], wd_[:, jj, dc * 128:(dc + 1) * 128], aT[:, jj, 0:gl],
                                     start=(jj == 0), stop=(jj == 3))
                            p.tt(acc[:, dc, q0:q0 + gl], acc[:, dc, q0:q0 + gl], psd[:, 0:gl], ALU.add)
                    ucnt += 1
            if is_cb:
                p.dma(PC_v.rearrange("k p t -> p k t"), acc[:, :, 0:bl])
            else:
                p.dma(PP_v[rr].rearrange("k p t -> p k t"), acc[:, :, 0:bl])
        p.pop()
        p.cc("ReduceScatter", ALU.add, PP, PS)
        p.cc("AllReduce", ALU.add, PC, PCS)

        p.push()
        PS_v = PS.rearrange("(k p) t -> k p t", k=8)
        PCS_v = PCS.rearrange("(k p) t -> k p t", k=8)
        modg = p.sb("modg", [128, 2, D])
        for r in range(2):
            p.dma(modg[:, r, :], MODS_all[4 * l + 2 + r:4 * l + 3 + r, 2048:3072].partition_broadcast(128))
        yTt = p.sb("yTt", [128, 8, 128])
        xm2 = p.sb("xm2", [128, D])
        xo = p.sb("xo", [128, D])
        for i in range(NT):
            r = 0 if i >= 2 else 1
            tok = slice(i * 128, (i + 1) * 128)
            if i >= 2:
                p.dma(yTt[:], PS_v[:, :, (i - 2) * 128:(i - 1) * 128].rearrange("k p t -> p k t"))
            else:
                p.dma(yTt[:], PCS_v[:, :, tok].rearrange("k p t -> p k t"))
            p.dma(xm2[:], XMID[tok, :])
            for half in range(2):
                for k in range(4):
                    dc = half * 4 + k
                    p.tr(F[half][:, k * 128:(k + 1) * 128], yTt[:, dc, :], ident_f)
                p.tt(xo[:, half * 512:(half + 1) * 512], F[half][:, :], modg[:, r, half * 512:(half + 1) * 512], ALU.mult)
            p.tt(xo[:], xo[:], xm2[:], ALU.add)
            p.dma(XCUR[tok, :], xo[:])
            if lat_last and i >= 2:
                p.dma(out[(i - 2) * 128:(i - 1) * 128, :], xo[:])
        p.pop()
    p.emit()
    return nc


def fused_in_maps(inp, TL, depth):
    S = TL * NCORE
    x = np.asarray(inp["x"], np.float32).reshape(S, D)
    ctx = np.ascontiguousarray(np.asarray(inp["ctx"], np.float32).reshape(TC, D))
    rope_all = rope_tables_np(S)
    A = lambda k: np.asarray(inp[k], np.float32)
    cc = np.stack([A("c").reshape(D), A("c_ctx").reshape(D)], 1)
    cc = np.ascontiguousarray(cc.reshape(8, 128, 2).transpose(1, 0, 2))
    common = {
        "ctx": ctx, "cc": cc,
        "g_norm1": A("g_norm1")[:depth], "g_norm2": A("g_norm2")[:depth],
        "w_in": A("w_in")[:depth], "w_uq": A("mla_w_uq")[:depth], "w_ukv": A("mla_w_ukv")[:depth],
        "small": np.concatenate([pack_small(inp, l) for l in range(depth)], 0),
        "conv_w": np.ascontiguousarray(A("ssm_conv_w")[:depth].reshape(depth, 5, 8, 128).transpose(0, 3, 2, 1)),
        "conv_b": np.ascontiguousarray(A("ssm_conv_b")[:depth].reshape(depth, 8, 128).transpose(0, 2, 1)),
        "consts": consts_np(),
        "lamc": np.array([[0.8 - 0.6 * math.exp(-0.3 * l), 1.0 - (0.8 - 0.6 * math.exp(-0.3 * l))]
                          for l in range(depth)], np.float32),
        "dif_lambda": A("dif_lambda")[:depth].reshape(depth, 256),
        "g_sub": A("dif_g_sub")[:depth].reshape(depth, 128, 1),
        "g_ssm": A("ssm_g_norm")[:depth], "b_gate": A("b_gate")[:depth],
        "w_up_mla": A("w_up_mla")[:depth], "w_up_dif": A("w_up_dif")[:depth], "w_up_ssm": A("w_up_ssm")[:depth],
        "w_out": A("w_out")[:depth], "w_router": A("moe_w_router")[:depth], "b_router": A("moe_b_router")[:depth],
    }
    wmod = A("w_mod")
    bmod = A("b_mod")
    maps = []
    for c in range(NCORE):
        s0, s1 = c * TL, (c + 1) * TL
        lm, hm_ = c // 2, c % 2
        m = dict(common)
        if lm < depth:
            m["wmod_sh"] = np.ascontiguousarray(wmod[lm][:, hm_ * 3072:(hm_ + 1) * 3072])
            m["bmod_sh"] = np.ascontiguousarray(bmod[lm][hm_ * 3072:(hm_ + 1) * 3072]).reshape(1, 3072)
        else:
            m["wmod_sh"] = np.zeros((D, 3072), np.float32)
            m["bmod_sh"] = np.zeros((1, 3072), np.float32)
        hmask = np.zeros((1, 4), np.float32)
        halosel = np.zeros((32, 128), np.float32)
        if c > 0:
            hmask[0, 0:2] = 1.0
            halosel[(c - 1) * 4 + 2, 0] = 1.0
            halosel[(c - 1) * 4 + 3, 1] = 1.0
        if c < NCORE - 1:
            hmask[0, 2:4] = 1.0
            halosel[(c + 1) * 4 + 0, 2] = 1.0
            halosel[(c + 1) * 4 + 1, 3] = 1.0
        cmask = np.zeros((1, 16), np.float32)
        cmask[0, 0:c] = 1.0
        cmask[0, 8 + c + 1:16] = 1.0
        sele = np.zeros((NEXP, 4, 128), np.float32)
        for i in range(4):
            sele[4 * c + i, i, :] = 1.0
        es = slice(4 * c, 4 * c + 4)
        bdp = np.zeros((depth, NEXP, D), np.float32)
        bdp[:, es, :] = A("moe_b_down")[:depth, es, :]
        m.update({
            "x": np.ascontiguousarray(x[s0:s1]), "rope": np.ascontiguousarray(rope_all[s0:s1]),
            "hmask": hmask, "halosel": halosel, "cmask": cmask, "sele": sele,
            "w_gu": np.ascontiguousarray(A("moe_w_gu")[:depth, es]),
            "w_down": np.ascontiguousarray(A("moe_w_down")[:depth, es]),
            "b_gu": np.ascontiguousarray(A("moe_b_gu")[:depth, es].reshape(depth, 4, 16, 128).transpose(0, 3, 1, 2)),
            "b_down_pad": bdp,
        })
        maps.append(m)
    return maps


_PROGS = {}


def run_fused(inp, TL, depth):
    key = (TL, depth)
    if key not in _PROGS:
        _PROGS[key] = build_fused(TL, depth)
    res = run_bass_kernel_spmd(_PROGS[key], fused_in_maps(inp, TL, depth), core_ids=list(range(NCORE))).results
    S = TL * NCORE
    return np.concatenate([np.asarray(r["out"]) for r in res], 0).reshape(1, S, D).astype(np.float32)


def kernel(**inputs):
    return run_fused(inputs, 2048, DEPTH)
```
